# Optimizing a Trainium2 kernel written in Bass

```python
import math
import jax
import jax.numpy as jnp
from jax import lax
import numpy as np

D_MODEL = 1024
BATCH = 4
SEQ = 4096
DEPTH = 4

CHUNK = 64
EPS = 1e-6
GLA_HEADS = 4
GLA_DK = D_MODEL // 8
GLA_DV = D_MODEL // 4
GLA_RANK = 16
GLA_TAU = 16.0
MLSTM_HEADS = 4
MLSTM_DK = D_MODEL // 8
MLSTM_DV = D_MODEL // 4
MLSTM_CONV = 4
SSD_HEAD_DIM = 64
SSD_INNER = D_MODEL
SSD_HEADS = SSD_INNER // SSD_HEAD_DIM
SSD_GROUPS = 2
SSD_STATE = 128
SSD_CONV = 4
D_FF = 2816
FFN_CONV = 3
N_BRANCH = 3

GLA_QK = GLA_HEADS * GLA_DK
GLA_V = GLA_HEADS * GLA_DV
MLSTM_QK = MLSTM_HEADS * MLSTM_DK
MLSTM_V = MLSTM_HEADS * MLSTM_DV
SSD_BC = SSD_GROUPS * SSD_STATE
SSD_CONV_DIM = SSD_INNER + 2 * SSD_BC
IN_SIZES = (GLA_QK, GLA_QK, GLA_V, GLA_RANK, GLA_V, 2 * MLSTM_QK, MLSTM_V, MLSTM_HEADS, MLSTM_HEADS, MLSTM_V, SSD_INNER, SSD_CONV_DIM, SSD_HEADS, N_BRANCH * D_MODEL)
D_IN = sum(IN_SIZES)
SPLIT_POINTS = tuple(int(s) for s in np.cumsum(IN_SIZES)[:-1])

kernel_name = 'hybrid_gla_mlstm_ssd_convffn'


def rmsnorm(x, g):
    xf = x.astype(jnp.float32)
    y = xf * lax.rsqrt(jnp.mean(xf * xf, axis=-1, keepdims=True) + EPS)
    return (y * g).astype(x.dtype)


def group_rmsnorm(x, g, n_groups):
    bsz, t, w = x.shape
    xf = x.astype(jnp.float32).reshape(bsz, t, n_groups, w // n_groups)
    y = xf * lax.rsqrt(jnp.mean(xf * xf, axis=-1, keepdims=True) + EPS)
    return (y.reshape(bsz, t, w) * g).astype(x.dtype)


def causal_dwconv(x, w, b):
    k = w.shape[0]
    y = lax.conv_general_dilated(x, w[:, None, :], window_strides=(1,), padding=[(k - 1, 0)],
                                 dimension_numbers=('NWC', 'WIO', 'NWC'), feature_group_count=x.shape[-1])
    return y + b


def heads(x, n):
    return x.reshape(x.shape[:-1] + (n, -1))


def to_chunks(x):
    bsz, t = x.shape[:2]
    x = x.reshape((bsz, t // CHUNK, CHUNK) + x.shape[2:])
    return jnp.moveaxis(x, 3, 1)


def from_chunks(y):
    y = jnp.moveaxis(y, 1, 3)
    return y.reshape((y.shape[0], y.shape[1] * y.shape[2]) + y.shape[3:])


def causal_mask():
    return jnp.tril(jnp.ones((CHUNK, CHUNK), dtype=bool))


def chunk_scan(d_state, decay):
    def step(s, inp):
        d_n, dec_n = inp
        return dec_n * s + d_n, s
    s0 = jnp.zeros_like(d_state[:, :, 0])
    _, s_prev = lax.scan(step, s0, (jnp.moveaxis(d_state, 2, 0), jnp.moveaxis(decay, 2, 0)))
    return jnp.moveaxis(s_prev, 0, 2)


def gla(q, k, v, log_a):
    out_dtype = q.dtype
    f32 = jnp.float32
    q = to_chunks(q.astype(f32)) * (GLA_DK ** -0.5)
    k = to_chunks(k.astype(f32))
    v = to_chunks(v.astype(f32))
    b = jnp.cumsum(to_chunks(log_a), axis=3)
    b_last = b[:, :, :, -1:, :]
    q_dec = q * jnp.exp(b)
    scores = jnp.einsum('bhncd,bhnsd->bhncs', q_dec, k * jnp.exp(-b))
    scores = jnp.where(causal_mask(), scores, 0.0)
    o = jnp.einsum('bhncs,bhnsv->bhncv', scores, v)
    d_state = jnp.einsum('bhncd,bhncv->bhndv', k * jnp.exp(b_last - b), v)
    s_prev = chunk_scan(d_state, jnp.exp(jnp.swapaxes(b_last, -1, -2)))
    o = o + jnp.einsum('bhncd,bhndv->bhncv', q_dec, s_prev)
    return from_chunks(o).astype(out_dtype)


def mlstm(q, k, v, i_pre, f_pre):
    out_dtype = q.dtype
    f32 = jnp.float32
    q = to_chunks(q.astype(f32))
    k = to_chunks(k.astype(f32)) * (MLSTM_DK ** -0.5)
    v = to_chunks(v.astype(f32))
    ig = to_chunks(i_pre)
    b = jnp.cumsum(to_chunks(jax.nn.log_sigmoid(f_pre)), axis=-1)
    b_last = b[..., -1]
    a = b_last[..., None] - b + ig
    m_loc = jnp.max(a, axis=-1)
    w = jnp.exp(a - m_loc[..., None])
    d_c = jnp.einsum('bhnc,bhncd,bhncv->bhndv', w, k, v)
    d_n = jnp.einsum('bhnc,bhncd->bhnd', w, k)

    def step(carry, inp):
        c, n, m = carry
        dc_i, dn_i, mloc_i, bl_i = inp
        m_new = jnp.maximum(bl_i + m, mloc_i)
        s_old = jnp.exp(bl_i + m - m_new)
        s_new = jnp.exp(mloc_i - m_new)
        c_new = s_old[..., None, None] * c + s_new[..., None, None] * dc_i
        n_new = s_old[..., None] * n + s_new[..., None] * dn_i
        return (c_new, n_new, m_new), (c, n, m)

    init = (jnp.zeros_like(d_c[:, :, 0]), jnp.zeros_like(d_n[:, :, 0]), jnp.zeros_like(m_loc[:, :, 0]))
    xs = (jnp.moveaxis(d_c, 2, 0), jnp.moveaxis(d_n, 2, 0), jnp.moveaxis(m_loc, 2, 0), jnp.moveaxis(b_last, 2, 0))
    _, (c_prev, n_prev, m_prev) = lax.scan(step, init, xs)
    c_prev = jnp.moveaxis(c_prev, 0, 2)
    n_prev = jnp.moveaxis(n_prev, 0, 2)
    m_prev = jnp.moveaxis(m_prev, 0, 2)

    log_d = jnp.where(causal_mask(), b[..., :, None] - b[..., None, :] + ig[..., None, :], -jnp.inf)
    m_inter = b + m_prev[..., None]
    m_t = jnp.maximum(m_inter, jnp.max(log_d, axis=-1))
    wts = jnp.einsum('bhncd,bhnsd->bhncs', q, k) * jnp.exp(log_d - m_t[..., None])
    s_inter = jnp.exp(m_inter - m_t)
    num = jnp.einsum('bhncs,bhnsv->bhncv', wts, v) + s_inter[..., None] * jnp.einsum('bhncd,bhndv->bhncv', q, c_prev)
    den = jnp.sum(wts, axis=-1) + s_inter * jnp.einsum('bhncd,bhnd->bhnc', q, n_prev)
    h = num / jnp.maximum(jnp.abs(den), jnp.exp(-m_t))[..., None]
    return from_chunks(h).astype(out_dtype)


def ssd(x, dt, a_neg, b_in, c_in):
    out_dtype = x.dtype
    f32 = jnp.float32
    bsz = x.shape[0]
    j = SSD_HEADS // SSD_GROUPS
    xc = to_chunks(x.astype(f32))
    nc = xc.shape[2]
    xc = xc.reshape(bsz, SSD_GROUPS, j, nc, CHUNK, SSD_HEAD_DIM)
    bc = to_chunks(b_in.astype(f32))
    cc = to_chunks(c_in.astype(f32))
    dtc = to_chunks(dt).reshape(bsz, SSD_GROUPS, j, nc, CHUNK)
    b = jnp.cumsum(dtc * a_neg.reshape(SSD_GROUPS, j)[None, :, :, None, None], axis=-1)
    b_last = b[..., -1]
    cb = jnp.einsum('bgncz,bgnsz->bgncs', cc, bc)
    decay = jnp.exp(jnp.where(causal_mask(), b[..., :, None] - b[..., None, :], -jnp.inf))
    mix = cb[:, :, None] * decay * dtc[..., None, :]
    y = jnp.einsum('bgjncs,bgjnsp->bgjncp', mix, xc)
    w = jnp.exp(b_last[..., None] - b) * dtc
    d_state = jnp.einsum('bgjnc,bgncz,bgjncp->bgjnzp', w, bc, xc)
    d_state = d_state.reshape(bsz, SSD_HEADS, nc, SSD_STATE, SSD_HEAD_DIM)
    s_prev = chunk_scan(d_state, jnp.exp(b_last).reshape(bsz, SSD_HEADS, nc, 1, 1))
    s_prev = s_prev.reshape(bsz, SSD_GROUPS, j, nc, SSD_STATE, SSD_HEAD_DIM)
    y = y + jnp.einsum('bgncz,bgjnzp->bgjncp', cc, s_prev) * jnp.exp(b)[..., None]
    y = y.reshape(bsz, SSD_HEADS, nc, CHUNK, SSD_HEAD_DIM)
    return from_chunks(y).astype(out_dtype)


def mixer_layer(u, w_in, gla_wa, gla_ba, gla_norm, mlstm_conv_w, mlstm_conv_b, mlstm_bi, mlstm_bf, mlstm_norm,
                ssd_conv_w, ssd_conv_b, ssd_dt_bias, ssd_a_log, ssd_d, ssd_norm, gate_b, w_branch, w_out):
    f32 = jnp.float32
    bsz, t, _ = u.shape
    proj = u @ w_in
    (gq, gk, gv, ga, gg, mqk, mv, mi, mf, mo, sz, sxbc, sdt, gates) = jnp.split(proj, SPLIT_POINTS, axis=-1)

    log_a = jax.nn.log_sigmoid((ga @ gla_wa + gla_ba).astype(f32)) / GLA_TAU
    o_gla = gla(heads(gq, GLA_HEADS), heads(gk, GLA_HEADS), heads(gv, GLA_HEADS), heads(log_a, GLA_HEADS))
    y_gla = group_rmsnorm(o_gla.reshape(bsz, t, GLA_V), gla_norm, GLA_HEADS) * jax.nn.silu(gg)

    mqk = jax.nn.silu(causal_dwconv(mqk, mlstm_conv_w, mlstm_conv_b))
    mq, mk = jnp.split(mqk, 2, axis=-1)
    o_m = mlstm(heads(mq, MLSTM_HEADS), heads(mk, MLSTM_HEADS), heads(mv, MLSTM_HEADS),
                (mi + mlstm_bi).astype(f32), (mf + mlstm_bf).astype(f32))
    y_m = group_rmsnorm(o_m.reshape(bsz, t, MLSTM_V), mlstm_norm, MLSTM_HEADS) * jax.nn.sigmoid(mo)

    xbc = jax.nn.silu(causal_dwconv(sxbc, ssd_conv_w, ssd_conv_b))
    sx, sb, sc = jnp.split(xbc, [SSD_INNER, SSD_INNER + SSD_BC], axis=-1)
    dt = jax.nn.softplus((sdt + ssd_dt_bias).astype(f32))
    a_neg = -jnp.exp(ssd_a_log.astype(f32))
    xh = heads(sx, SSD_HEADS)
    y = ssd(xh, dt, a_neg, heads(sb, SSD_GROUPS), heads(sc, SSD_GROUPS))
    y = (y + ssd_d[:, None] * xh).reshape(bsz, t, SSD_INNER) * jax.nn.silu(sz)
    y_s = group_rmsnorm(y, ssd_norm, SSD_GROUPS)

    ys = jnp.stack([y_gla, y_m, y_s], axis=2)
    z = jnp.einsum('btkw,kwd->btkd', ys, w_branch)
    g = jax.nn.sigmoid(gates + gate_b).reshape(bsz, t, N_BRANCH, D_MODEL)
    return jnp.sum(g * z, axis=2) @ w_out


def conv_ffn(u, w_up, conv_w, conv_b, w_down):
    a = causal_dwconv(u @ w_up, conv_w, conv_b)
    gate, val = jnp.split(a, 2, axis=-1)
    return (jax.nn.silu(gate) * val) @ w_down


def setup_inputs(seed: int = 0) -> dict:
    key = jax.random.key(seed)
    ks = jax.random.split(key, 32)
    f32 = jnp.float32
    L = DEPTH
    res_scale = (2.0 * L) ** -0.5

    def nrm(k, shape, scale):
        return jax.random.normal(k, shape, f32) * scale

    dt0 = jnp.exp(jax.random.uniform(ks[15], (L, SSD_HEADS), f32, math.log(1e-3), math.log(1e-1)))
    return {
        'x': nrm(ks[0], (BATCH, SEQ, D_MODEL), 1.0),
        'norm_mix': 1.0 + nrm(ks[1], (L, D_MODEL), 0.02),
        'w_in': nrm(ks[2], (L, D_MODEL, D_IN), D_MODEL ** -0.5),
        'gla_wa': nrm(ks[3], (L, GLA_RANK, GLA_QK), GLA_RANK ** -0.5),
        'gla_ba': nrm(ks[4], (L, GLA_QK), 0.1),
        'gla_norm': 1.0 + nrm(ks[5], (L, GLA_V), 0.02),
        'mlstm_conv_w': nrm(ks[6], (L, MLSTM_CONV, 2 * MLSTM_QK), MLSTM_CONV ** -0.5),
        'mlstm_conv_b': nrm(ks[7], (L, 2 * MLSTM_QK), 0.02),
        'mlstm_bi': nrm(ks[8], (L, MLSTM_HEADS), 0.1),
        'mlstm_bf': jnp.linspace(3.0, 6.0, MLSTM_HEADS, dtype=f32)[None, :] + nrm(ks[9], (L, MLSTM_HEADS), 0.1),
        'mlstm_norm': 1.0 + nrm(ks[10], (L, MLSTM_V), 0.02),
        'ssd_conv_w': nrm(ks[11], (L, SSD_CONV, SSD_CONV_DIM), SSD_CONV ** -0.5),
        'ssd_conv_b': nrm(ks[12], (L, SSD_CONV_DIM), 0.02),
        'ssd_dt_bias': dt0 + jnp.log(-jnp.expm1(-dt0)),
        'ssd_a_log': jnp.log(jax.random.uniform(ks[13], (L, SSD_HEADS), f32, 1.0, 16.0)),
        'ssd_d': 1.0 + nrm(ks[14], (L, SSD_HEADS), 0.1),
        'ssd_norm': 1.0 + nrm(ks[16], (L, SSD_INNER), 0.02),
        'gate_b': nrm(ks[17], (L, N_BRANCH * D_MODEL), 0.1),
        'w_branch': nrm(ks[18], (L, N_BRANCH, D_MODEL, D_MODEL), D_MODEL ** -0.5),
        'w_out': nrm(ks[19], (L, D_MODEL, D_MODEL), D_MODEL ** -0.5 * res_scale),
        'norm_ffn': 1.0 + nrm(ks[20], (L, D_MODEL), 0.02),
        'w_up': nrm(ks[21], (L, D_MODEL, 2 * D_FF), D_MODEL ** -0.5),
        'ffn_conv_w': nrm(ks[22], (L, FFN_CONV, 2 * D_FF), FFN_CONV ** -0.5),
        'ffn_conv_b': nrm(ks[23], (L, 2 * D_FF), 0.02),
        'w_down': nrm(ks[24], (L, D_FF, D_MODEL), D_FF ** -0.5 * res_scale),
        'norm_final': 1.0 + nrm(ks[25], (D_MODEL,), 0.02),
    }


def reference(x, norm_mix, w_in, gla_wa, gla_ba, gla_norm, mlstm_conv_w, mlstm_conv_b, mlstm_bi, mlstm_bf,
              mlstm_norm, ssd_conv_w, ssd_conv_b, ssd_dt_bias, ssd_a_log, ssd_d, ssd_norm, gate_b, w_branch,
              w_out, norm_ffn, w_up, ffn_conv_w, ffn_conv_b, w_down, norm_final):
    h = x
    for l in range(DEPTH):
        h = h + mixer_layer(rmsnorm(h, norm_mix[l]), w_in[l], gla_wa[l], gla_ba[l], gla_norm[l],
                            mlstm_conv_w[l], mlstm_conv_b[l], mlstm_bi[l], mlstm_bf[l], mlstm_norm[l],
                            ssd_conv_w[l], ssd_conv_b[l], ssd_dt_bias[l], ssd_a_log[l], ssd_d[l], ssd_norm[l],
                            gate_b[l], w_branch[l], w_out[l])
        h = h + conv_ffn(rmsnorm(h, norm_ffn[l]), w_up[l], ffn_conv_w[l], ffn_conv_b[l], w_down[l])
    return rmsnorm(h, norm_final)
```

```python
import numpy as np
from collections import deque
from contextlib import ExitStack
import concourse.bass as bass
import concourse.mybir as mybir
from concourse.bass_utils import run_bass_kernel_spmd

F32 = mybir.dt.float32
BF16 = mybir.dt.bfloat16
AF = mybir.ActivationFunctionType
ALU = mybir.AluOpType
AX = mybir.AxisListType

D = 1024
DIN = 11816
DFF = 2816
EPS = 1e-6
O_GQ, O_GK, O_GV, O_GA, O_GG = 0, 512, 1024, 2048, 2064
O_MQK, O_MV, O_MI, O_MO = 3088, 4112, 5136, 5144
O_SZ, O_SXBC, O_SDT, O_GATES = 6168, 7192, 8728, 8744
P_NMIX, P_NFFN, P_GNW, P_MNW, P_SNW, P_MCW, P_MCB, P_SCW, P_SCB, P_FCW, P_FCB, P_GB, NPP = \
    0, 8, 16, 24, 32, 40, 72, 80, 128, 140, 272, 316, 340
B_BI, B_DTB, B_ALOG, B_SD, NBP = 0, 8, 24, 40, 56
C_TRI, C_UGT, C_TRIG, C_UG, C_UNEG, C_ONES, C_ONESNEG, C_MASKM, NCONST = 0, 1, 2, 3, 4, 5, 6, 7, 8


class Buf:
    __slots__ = ("lw", "rd")

    def __init__(self):
        self.lw = None
        self.rd = []


class TK:
    def __init__(self, nc, es, n_dma_sems=12):
        self.nc = nc
        self.eng = {"pe": nc.tensor, "act": nc.scalar, "dve": nc.vector, "pool": nc.gpsimd, "sp": nc.sync}
        self.sem = {}
        self.cnt = {}
        for e in ["pe", "act", "dve", "pool"]:
            self.sem[e] = es.enter_context(nc.semaphore("s_" + e))
            self.cnt[e] = 0
        self.dsem = [es.enter_context(nc.semaphore("d%d" % i)) for i in range(n_dma_sems)]
        self.dcnt = [0] * n_dma_sems
        self.dnext = 0
        self.waited = {}
        self.dry = False
        self.nins = 0
        self.dry_count = 0
        self.defer_q = None
        self.pending = None
        self.pop_every = 1
        self.b_count = 0

    def _wait(self, e, dep):
        key, val = dep
        if key == e and e == "pe":
            return
        w = self.waited.setdefault(e, {})
        if w.get(key, 0) >= val:
            return
        w[key] = val
        sem = self.sem[key] if isinstance(key, str) else self.dsem[key]
        self.eng[e].wait_ge(sem, val)
        self.nins += 1

    def _deps(self, e, reads, writes):
        for b in reads:
            if b.lw is not None:
                self._wait(e, b.lw)
        for b in writes:
            if b.lw is not None:
                self._wait(e, b.lw)
            for r in b.rd:
                self._wait(e, r)

    def _mark(self, tag, reads, writes):
        for b in reads:
            b.rd.append(tag)
            if len(b.rd) > 64:
                last = {}
                for k, v in b.rd:
                    last[k] = max(last.get(k, 0), v)
                b.rd = list(last.items())
        for b in writes:
            b.lw = tag
            b.rd = []

    def _tick(self):
        if self.pending:
            self.b_count += 1
            if self.b_count >= self.pop_every:
                self.b_count = 0
                self.pending.popleft()()

    def op(self, e, fn, reads=(), writes=()):
        if self.dry:
            self.dry_count += 1
            return
        if self.defer_q is not None:
            self.defer_q.append(lambda: self._op(e, fn, reads, writes))
            return
        self._op(e, fn, reads, writes)
        self._tick()

    def dma(self, e, out, in_, reads=(), writes=()):
        if self.dry:
            self.dry_count += 1
            return
        if self.defer_q is not None:
            self.defer_q.append(lambda: self._dma(e, out, in_, reads, writes))
            return
        self._dma(e, out, in_, reads, writes)
        self._tick()

    def _op(self, e, fn, reads=(), writes=()):
        self._deps(e, reads, writes)
        ins = fn(self.eng[e])
        self.cnt[e] += 1
        self.nins += 1
        ins.then_inc(self.sem[e], 1)
        self._mark((e, self.cnt[e]), reads, writes)

    def _dma(self, e, out, in_, reads=(), writes=()):
        i = self.dnext
        self.dnext = (self.dnext + 1) % len(self.dsem)
        if self.dcnt[i] > 0:
            self._wait(e, (i, self.dcnt[i]))
        self._deps(e, reads, writes)
        ins = self.eng[e].dma_start(out=out, in_=in_)
        self.nins += 1
        self.dcnt[i] += 16
        ins.then_inc(self.dsem[i], 16)
        self._mark((i, self.dcnt[i]), reads, writes)


def build_program(NL, NTILES, NCH=2, NSLAB=3, LOOK=2):
    pass


def _bp(NL, NTILES, NCH=2, NSLAB=4, LOOK=2):
    TT = NCH * 128
    T = NTILES * TT
    nc = bass.Bass("TRN2", target_bir_lowering=False)
    dr = lambda name, shape, kind="ExternalInput": nc.dram_tensor(name, shape, F32, kind=kind).ap()
    x_d = dr("x", [T, D])
    win_d = dr("w_in", [NL, D, DIN])
    wbr_d = dr("w_branch", [NL, 3, D, D])
    wout_d = dr("w_out", [NL, D, D])
    wup_d = dr("w_up", [NL, D, 2 * DFF])
    wdn_d = dr("w_down", [NL, DFF, D])
    wab_d = dr("gla_wab", [NL, 17, 512])
    pp_d = dr("pp", [NL, 128, NPP])
    bp_d = dr("bp", [NL, 128, NBP])
    nf_d = dr("nf", [128, D])
    cst_d = dr("consts", [128, NCONST * 128])
    idn_d = dr("ident", [128, 128])
    out_d = dr("out", [T, D], kind="ExternalOutput")
    DBG = _bp.debug
    dbg_d = dr("dbg", [24, 128, 2048], kind="ExternalOutput") if DBG else None

    with ExitStack() as es:
        tk = TK(nc, es)
        _n = [0]

        def sb(shape, dt):
            _n[0] += 1
            return es.enter_context(nc.sbuf_tensor("t%d" % _n[0], shape, dt))

        hres = sb([128, NCH, D], F32); b_h = Buf()
        uT = sb([128, 8, TT], BF16); b_uT = Buf()
        xn = sb([128, D], BF16); b_xn = Buf()
        _gS1 = sb([128, 4, 256], F32); _bgS1 = Buf()
        _mC1 = sb([128, 4, 256], F32); _bmC1 = Buf()
        _sS1 = sb([128, 1024], F32); _bsS1 = Buf()
        gS = [_gS1] * NL; b_gS = [_bgS1] * NL
        mC = [_mC1] * NL; b_mC = [_bmC1] * NL
        gS_d = nc.dram_tensor("gS_d", [NL, 128, 1024], F32).ap(); b_gSd = [Buf() for _ in range(NL)]
        mC_d = nc.dram_tensor("mC_d", [NL, 128, 1024], F32).ap(); b_mCd = [Buf() for _ in range(NL)]
        sS_d = nc.dram_tensor("sS_d", [NL, 128, 1024], F32).ap(); b_sSd = [Buf() for _ in range(NL)]
        mN = [sb([128, 4], F32) for _ in range(NL)]; b_mN = [Buf() for _ in range(NL)]
        sS = [_sS1] * NL; b_sS = [_bsS1] * NL
        gSb = sb([128, 4, 256], BF16); b_gSb = Buf()
        mCb = sb([128, 4, 256], BF16); b_mCb = Buf()
        mNb = sb([128, 4], BF16); b_mNb = Buf()
        sSb = sb([128, 1024], BF16); b_sSb = Buf()
        mh = [sb([128, 8, 3], F32) for _ in range(NL)]; b_mh = [Buf() for _ in range(NL)]
        sh = [sb([128, 12, 3], F32) for _ in range(NL)]; b_sh = [Buf() for _ in range(NL)]
        fh = [sb([128, 44, 2], F32) for _ in range(NL)]; b_fh = [Buf() for _ in range(NL)]
        pp = sb([128, NL, NPP], F32); b_pp = Buf()
        bp = sb([128, NL, NBP], F32); b_bp = Buf()
        cst = sb([128, NCONST, 128], F32); b_cst = Buf()
        idf = sb([128, 128], F32); b_idf = Buf()
        idb = sb([128, 128], BF16); b_idb = Buf()
        onesb = sb([128, 4], BF16); b_onesb = Buf()
        slabs = [sb([128, 8, 512], BF16) for _ in range(NSLAB)]; b_slab = [Buf() for _ in range(NSLAB)]
        wab = sb([32, 512], F32); b_wab = Buf()
        gaT = sb([32, TT], F32); b_gaT = Buf()
        aneg = sb([128, 16], F32); b_aneg = Buf()
        A8 = [sb([128, 8, TT], BF16) for _ in range(3)]; b_A8 = [Buf() for _ in range(3)]
        qdT = sb([128, 4, TT], BF16); b_qdT = Buf()
        kiT = sb([128, 4, TT], BF16); b_kiT = Buf()
        qT2 = sb([128, 4, TT], BF16); b_qT2 = Buf()
        kT2 = sb([128, 4, TT], BF16); b_kT2 = Buf()
        TG2 = sb([128, 8, TT], BF16); b_TG2 = Buf()
        YT2 = sb([128, 8, TT], BF16); b_YT2 = Buf()
        VB2 = sb([128, NCH, 1024], BF16); b_VB2 = Buf()
        cbm_t = sb([128, 256], F32); b_cbm_t = Buf()
        sm2 = sb([128, NCH, 64], F32); b_sm2 = Buf()
        la = sb([128, NCH, 512], F32); b_la = Buf()
        Epos = sb([128, 4, TT], F32); b_Epos = Buf()
        Eneg = sb([128, 4, TT], F32); b_Eneg = Buf()
        merged = sb([128, 8, TT], F32); b_merged = Buf()
        vb = sb([128, NCH, 1024], BF16); b_vb = Buf()
        kdec = sb([128, NCH, 512], BF16); b_kdec = Buf()
        erev = sb([128, NCH, 512], F32); b_erev = Buf()
        BT = sb([128, 2, TT], BF16); b_BT = Buf()
        CT = sb([128, 2, TT], BF16); b_CT = Buf()
        szs = sb([128, NCH, 1024], BF16); b_szs = Buf()
        sm = sb([128, NCH, 64], F32); b_sm = Buf()
        st = sb([128, 64], F32); b_st = Buf()
        F4 = [sb([128, 1024], F32) for _ in range(4)]; b_F4 = [Buf() for _ in range(4)]
        B2 = [sb([128, 1024], BF16) for _ in range(6)]; b_B2 = [Buf() for _ in range(6)]
        mixT = sb([128, 16, 128], BF16); b_mixT = Buf()
        xs = [sb([128, TT + 4], F32) for _ in range(2)]; b_xs = [Buf() for _ in range(2)]
        SG = [sb([128, 512], F32) for _ in range(3)]; b_SG = [Buf() for _ in range(3)]
        ngb = sb([128, NL, 24], F32); b_ngb = Buf()
        acc = [sb([128, TT], F32) for _ in range(3)]; b_acc = [Buf() for _ in range(3)]
        psf = es.enter_context(nc.psum_tensor("psf", [128, 6, 512], F32)); b_psf = [Buf() for _ in range(6)]
        psb = es.enter_context(nc.psum_tensor("psb", [128, 2, 1024], BF16)); b_psb = [Buf() for _ in range(2)]
        rr = {"p1": 0, "a1": 0, "pb": 0, "xs": 0, "acc": 0, "sg": 0}

        def pbank():
            i = rr["p1"]; rr["p1"] = (i + 1) % 2
            return psf[:, i, :], [b_psf[i]]

        def pbankA():
            i = rr["a1"]; rr["a1"] = (i + 1) % 2
            return psf[:, 2 + i, :], [b_psf[2 + i]]

        def pbank2():
            return psf[:, 4:6, :], [b_psf[4], b_psf[5]]

        bg = [None]

        def bg_step():
            g = bg[0]
            if g is not None:
                try:
                    next(g)
                except StopIteration:
                    bg[0] = None

        bg_counts = []
        bg_idx = [0]

        def bg_run(gen, fn):
            if tk.dry:
                c0 = tk.dry_count
                for _ in gen:
                    pass
                nA = tk.dry_count - c0
                c0 = tk.dry_count
                fn()
                bg_counts.append((nA, tk.dry_count - c0))
                return
            nA, nB = bg_counts[bg_idx[0]]
            bg_idx[0] += 1
            tk.defer_q = []
            for _ in gen:
                pass
            q = tk.defer_q
            tk.defer_q = None
            tk.pending = deque(q)
            tk.pop_every = 1
            tk.b_count = 0
            fn()
            while tk.pending:
                tk.pending.popleft()()
            tk.pending = None

        def pbf():
            i = rr["pb"]; rr["pb"] = (i + 1) % 2
            return psb[:, i, :], [b_psb[i]]

        def CST(i):
            return cst[:, i, :]

        dbgbuf = sb([128, 2048], F32) if DBG else None
        b_dbg = Buf()

        def dump(idx, ap, rbufs, n):
            if not DBG:
                return
            cp(dbgbuf[:, 0:n], ap, rbufs, [b_dbg])
            tk.dma("sp", dbg_d[idx, :, 0:n], dbgbuf[:, 0:n], reads=[b_dbg])

        slab_specs = []
        slab_state = {"next": 0, "issued": 0}

        def issue_slab(i):
            src, nk, n = slab_specs[i]
            bi = i % NSLAB
            tk.dma("pool", slabs[bi][:, 0:nk, 0:n], src.rearrange("(k p) n -> p k n", p=128), writes=[b_slab[bi]])

        def slab(src, nk, n):
            if tk.dry:
                slab_specs.append((src, nk, n))
                return slabs[0], b_slab[0]
            i = slab_state["next"]; slab_state["next"] += 1
            lim = min(i + LOOK, len(slab_specs) - 1)
            while slab_state["issued"] <= lim:
                issue_slab(slab_state["issued"]); slab_state["issued"] += 1
            return slabs[i % NSLAB], b_slab[i % NSLAB]

        def mm(out, lhsT, rhs, start, stop, reads, writes):
            tk.op("pe", lambda e: e.matmul(out, lhsT=lhsT, rhs=rhs, start=start, stop=stop), reads=reads, writes=writes)

        def act(out, in_, func, reads, writes, scale=1.0, bias=None, accum=None):
            kw = {}
            if bias is not None:
                kw["bias"] = bias
            if accum is not None:
                kw["accum_out"] = accum
            tk.op("act", lambda e: e.activation(out=out, in_=in_, func=func, scale=scale, **kw), reads=reads, writes=writes)

        def tt(out, in0, in1, op, reads, writes):
            tk.op("dve", lambda e: e.tensor_tensor(out=out, in0=in0, in1=in1, op=op), reads=reads, writes=writes)

        def stt(out, in0, scalar, in1, op0, op1, reads, writes):
            tk.op("dve", lambda e: e.scalar_tensor_tensor(out=out, in0=in0, scalar=scalar, in1=in1, op0=op0, op1=op1),
                  reads=reads, writes=writes)

        def ts(out, in0, s1, s2, op0, op1, reads, writes):
            if s2 is None:
                tk.op("dve", lambda e: e.tensor_scalar(out=out, in0=in0, scalar1=s1, scalar2=0.0, op0=op0, op1=ALU.add), reads=reads, writes=writes)
            else:
                tk.op("dve", lambda e: e.tensor_scalar(out=out, in0=in0, scalar1=s1, scalar2=s2, op0=op0, op1=op1),
                      reads=reads, writes=writes)

        def trp(out, in_, reads, writes):
            tk.op("pe", lambda e: e.transpose(out=out, in_=in_, identity=idb[:]), reads=reads, writes=writes)

        def rsum(out, in_, reads, writes):
            tk.op("dve", lambda e: e.reduce_sum(out=out, in_=in_, axis=AX.X), reads=reads, writes=writes)

        def recip(out, in_, reads, writes):
            tk.op("dve", lambda e: e.reciprocal(out=out, in_=in_), reads=reads, writes=writes)

        def cp(out, in_, reads, writes):
            tk.op("dve", lambda e: e.tensor_copy(out=out, in_=in_), reads=reads, writes=writes)

        def sig3(src, n, reads, nbias=None, nbias_reads=(), final=None, final_w=None):
            i = rr["sg"]; rr["sg"] = (i + 1) % 3
            S, bS = SG[i], b_SG[i]
            if nbias is None:
                act(S[:, 0:n], src, AF.Exp, reads, [bS], scale=-1.0)
            else:
                act(S[:, 0:n], src, AF.Exp, reads + list(nbias_reads), [bS], scale=-1.0, bias=nbias)
            act(S[:, 0:n], S[:, 0:n], AF.Ln, [bS], [bS], bias=1.0)
            if final is None:
                act(S[:, 0:n], S[:, 0:n], AF.Exp, [bS], [bS], scale=-1.0)
                return S[:, 0:n], bS
            act(final, S[:, 0:n], AF.Exp, [bS], final_w, scale=-1.0)
            return None, None

        def proj_fm(wsrc, col0, nchunks, consume, rhsT=None, rb=None):
            rhsT = uT if rhsT is None else rhsT
            rb = b_uT if rb is None else rb
            j = 0
            while j < nchunks:
                n = min(4, nchunks - j)
                sl, bs = slab(wsrc[:, col0 + j * 128: col0 + (j + n) * 128], 8, n * 128)
                for jj in range(n):
                    ps, bps = pbank()
                    for kd in range(8):
                        mm(ps[:, 0:TT], sl[:, kd, jj * 128:(jj + 1) * 128], rhsT[:, kd, :], kd == 0, kd == 7,
                           [bs, rb], bps)
                    consume(j + jj, ps[:, 0:TT], bps)
                    bg_step()
                j += n

        def proj_tm(wsrc, col0, ncols, consume):
            s = 0
            while s * 512 < ncols:
                n = min(512, ncols - s * 512)
                sl, bs = slab(wsrc[:, col0 + s * 512: col0 + s * 512 + n], 8, n)
                for c in range(NCH):
                    ps, bps = pbank()
                    for kd in range(8):
                        mm(ps[:, 0:n], uT[:, kd, c * 128:(c + 1) * 128], sl[:, kd, 0:n], kd == 0, kd == 7, [bs, b_uT], bps)
                    consume(s, c, ps[:, 0:n], bps)
                    bg_step()
                s += 1

        def rstd_from_ss(ss_ap, n, width, reads):
            o = st[:, 32:32 + n]
            ts(o, ss_ap, 1.0 / width, EPS, ALU.mult, ALU.add, reads + [b_st], [b_st])
            act(o, o, AF.Ln, [b_st], [b_st])
            act(o, o, AF.Exp, [b_st], [b_st], scale=-0.5)
            return o

        def rmsnorm_to_uT(l, pcol):
            for c in range(NCH):
                tk.op("dve", lambda e: e.memset(st[:, 0:1], 0.0), writes=[b_st])
                act(F4[0][:], hres[:, c, :], AF.Square, [b_h], [b_F4[0], b_st], accum=st[:, 0:1])
                r = rstd_from_ss(st[:, 0:1], 1, D, [])
                act(xn[:], hres[:, c, :], AF.Identity, [b_h, b_st], [b_xn], scale=r)
                pt, bpt = pbf()
                for kd in range(8):
                    tk.op("pe", lambda e: e.transpose(out=pt[:, kd * 128:(kd + 1) * 128], in_=xn[:, kd * 128:(kd + 1) * 128],
                                                      identity=idb[:]), reads=[b_xn, b_idb], writes=bpt)
                tt(uT[:, :, c * 128:(c + 1) * 128], pt.rearrange("p (k t) -> p k t", k=8),
                   pp[:, l, pcol:pcol + 8].unsqueeze(2).broadcast_to([128, 8, 128]), ALU.mult, bpt + [b_pp], [b_uT])

        def conv(ps, bps, hal, b_hal, j, K, wbase, bcol, l):
            i = rr["xs"]; rr["xs"] = (i + 1) % 2
            a = rr["acc"]; rr["acc"] = (a + 1) % 3
            X, bX = xs[i], b_xs[i]
            A, bA = acc[a], b_acc[a]
            act(X[:, K - 1:K - 1 + TT], ps, AF.Copy, bps, [bX])
            cp(X[:, 0:K - 1], hal[:, j, 0:K - 1], [b_hal], [bX])
            cp(hal[:, j, 0:K - 1], X[:, TT:TT + K - 1], [bX], [b_hal])
            act(A[:], ps, AF.Identity, bps + [b_pp], [bA], scale=pp[:, l, wbase + K - 1:wbase + K],
                bias=pp[:, l, bcol:bcol + 1])
            for k in range(K - 1):
                stt(A[:], X[:, k:k + TT], pp[:, l, wbase + k:wbase + k + 1], A[:], ALU.mult, ALU.add, [bX, bA, b_pp], [bA])
            return A, bA

        def norm_transpose_out(num_ps, bnum, fac, nh, tgate, b_tg, c, extra_reads, yT, b_yT):
            on, b_on = B2[2], b_B2[2]
            w = 1024 // nh
            tt(on[:].rearrange("p (h v) -> p h v", h=nh), num_ps.rearrange("p (h v) -> p h v", h=nh),
               fac.unsqueeze(2).broadcast_to([128, nh, w]), ALU.mult, bnum + extra_reads, [b_on])
            pt, bpt = pbf()
            for j in range(8):
                trp(pt[:, j * 128:(j + 1) * 128], on[:, j * 128:(j + 1) * 128], [b_on, b_idb], bpt)
            tt(yT[:, :, c * 128:(c + 1) * 128], pt.rearrange("p (k t) -> p k t", k=8), tgate, ALU.mult,
               bpt + b_tg, [b_yT])

        def branch(l, k, yTs, b_yTs):
            gT, b_gT = A8[1], b_A8[1]

            def cons_g(j, ps, bps):
                sig3(ps, TT, bps, nbias=ngb[:, l, k * 8 + j:k * 8 + j + 1], nbias_reads=[b_ngb], final=gT[:, j, :], final_w=[b_gT])
            proj_fm(win_d[l], O_GATES + k * 1024, 8, cons_g)

            def cons_z(j, ps, bps):
                if k == 0:
                    tt(merged[:, j, :], ps, gT[:, j, :], ALU.mult, bps + [b_gT], [b_merged])
                else:
                    tmp, b_tmp = acc[0], b_acc[0]
                    tt(tmp[:], ps, gT[:, j, :], ALU.mult, bps + [b_gT], [b_tmp])
                    tt(merged[:, j, :], merged[:, j, :], tmp[:], ALU.add, [b_merged, b_tmp], [b_merged])
            proj_fm(wbr_d[l, k], 0, 8, cons_z, rhsT=yTs, rb=b_yTs)

        def gla_proj(l):
            sl, bs = slab(win_d[l][:, O_GA:O_GA + 16], 8, 16)
            ps, bps = pbank()
            for kd in range(8):
                mm(ps[0:16, 0:TT], sl[:, kd, 0:16], uT[:, kd, :], kd == 0, kd == 7, [bs, b_uT], bps)
            act(gaT[0:16, :], ps[0:16, 0:TT], AF.Copy, bps, [b_gaT])
            tk.dma("sp", wab[0:17, :], wab_d[l], writes=[b_wab])
            for c in range(NCH):
                ps, bps = pbank()
                mm(ps[:, :], gaT[0:17, c * 128:(c + 1) * 128], wab[0:17, :], True, True, [b_gaT, b_wab], bps)
                act(F4[1][:, 0:512], ps, AF.Exp, bps, [b_F4[1]], scale=-1.0)
                act(la[:, c, :], F4[1][:, 0:512], AF.Ln, [b_F4[1]], [b_la], bias=1.0)
            for h in range(4):
                ps, bps = pbank()
                for c in range(NCH):
                    mm(ps[:, c * 128:(c + 1) * 128], la[:, c, h * 128:(h + 1) * 128], CST(C_TRIG), True, True,
                       [b_la, b_cst], bps)
                act(Epos[:, h, :], ps[:, 0:TT], AF.Exp, bps, [b_Epos])
                act(Eneg[:, h, :], ps[:, 0:TT], AF.Exp, bps, [b_Eneg], scale=-1.0)

            def cons_q(j, ps, bps):
                stt(qdT[:, j, :], ps, 128.0 ** -0.5, Epos[:, j, :], ALU.mult, ALU.mult, bps + [b_Epos], [b_qdT])
            proj_fm(win_d[l], O_GQ, 4, cons_q)

            def cons_k(j, ps, bps):
                tt(kiT[:, j, :], ps, Eneg[:, j, :], ALU.mult, bps + [b_Eneg], [b_kiT])
            proj_fm(win_d[l], O_GK, 4, cons_k)

            tg, b_tg = A8[0], b_A8[0]

            def cons_gg(j, ps, bps):
                S, bS = sig3(ps, TT, bps)
                stt(tg[:, j, :], ps, pp[:, l, P_GNW + j:P_GNW + j + 1], S, ALU.mult, ALU.mult, bps + [bS, b_pp], [b_tg])
            proj_fm(win_d[l], O_GG, 8, cons_gg)

            for c in range(NCH):
                ps, bps = pbank()
                mm(ps[:, :], CST(C_UG), la[:, c, :], True, True, [b_la, b_cst], bps)
                act(erev[:, c, :], ps, AF.Exp, bps, [b_erev])

            def cons_ktm(s, c, ps, bps):
                tt(kdec[:, c, :], ps, erev[:, c, :], ALU.mult, bps + [b_erev], [b_kdec])
            proj_tm(win_d[l], O_GK, 512, cons_ktm)

            def cons_v(s, c, ps, bps):
                act(vb[:, c, s * 512:(s + 1) * 512], ps, AF.Copy, bps, [b_vb])
            proj_tm(win_d[l], O_GV, 1024, cons_v)

        def gla_loop(l):
            tg, b_tg = A8[0], b_A8[0]
            tk.dma("sp", gS[l][:].rearrange("p h v -> p (h v)"), gS_d[l], reads=[b_gSd[l]], writes=[b_gS[l]])
            act(gSb[:], gS[l][:], AF.Copy, [b_gS[l]], [b_gSb])
            yield
            for c in range(NCH):
                cs = slice(c * 128, (c + 1) * 128)
                ps, bps = pbankA()
                for h in range(4):
                    mm(ps[:, h * 128:(h + 1) * 128], kiT[:, h, cs], qdT[:, h, cs], True, True, [b_kiT, b_qdT], bps)
                scT, b_scT = B2[0], b_B2[0]
                tt(scT[:, 0:512].rearrange("p (h c) -> p h c", h=4), ps.rearrange("p (h c) -> p h c", h=4),
                   CST(C_TRI).unsqueeze(1).broadcast_to([128, 4, 128]), ALU.mult, bps + [b_cst], [b_scT])
                yield
                po, bpo = pbank2()
                pof = po.rearrange("p a b -> p (a b)")
                for h in range(4):
                    mm(pof[:, h * 256:(h + 1) * 256], scT[:, h * 128:(h + 1) * 128], vb[:, c, h * 256:(h + 1) * 256], True, False,
                       [b_scT, b_vb], bpo)
                    mm(pof[:, h * 256:(h + 1) * 256], qdT[:, h, cs], gSb[:, h, :], False, True, [b_qdT, b_gSb], bpo)
                yield
                act(F4[0][:], pof, AF.Square, bpo, [b_F4[0]])
                rsum(st[:, 0:4], F4[0][:].rearrange("p (h v) -> p h v", h=4), [b_F4[0]], [b_st])
                yield
                r = rstd_from_ss(st[:, 0:4], 4, 256, [])
                yield
                norm_transpose_out(pof, bpo, r, 4, tg[:, :, cs], [b_tg], c, [b_st], A8[2], b_A8[2])
                yield
                pst, bpst = pbank2()
                pstf = pst.rearrange("p a b -> p (a b)")
                for h in range(4):
                    mm(pstf[:, h * 256:(h + 1) * 256], kdec[:, c, h * 128:(h + 1) * 128], vb[:, c, h * 256:(h + 1) * 256],
                       True, True, [b_kdec, b_vb], bpst)
                yield
                for h in range(4):
                    stt(gS[l][:, h, :], gS[l][:, h, :], Epos[:, h, c * 128 + 127:c * 128 + 128], pstf[:, h * 256:(h + 1) * 256],
                        ALU.mult, ALU.add, [b_gS[l], b_Epos] + bpst, [b_gS[l]])
                act(gSb[:], gS[l][:], AF.Copy, [b_gS[l]], [b_gSb])
                yield
            tk.dma("sp", gS_d[l], gS[l][:].rearrange("p h v -> p (h v)"), reads=[b_gS[l]], writes=[b_gSd[l]])

        def mlstm_proj(l):
            qT, b_qT, kT, b_kT = qT2, b_qT2, kT2, b_kT2

            def cons_qk(j, ps, bps):
                A, bA = conv(ps, bps, mh[l], b_mh[l], j, 4, P_MCW + j * 4, P_MCB + j, l)
                S, bS = sig3(A[:], TT, [bA])
                if j < 4:
                    tt(qT[:, j, :], A[:], S, ALU.mult, [bA, bS], [b_qT])
                else:
                    tt(kT[:, j - 4, :], A[:], S, ALU.mult, [bA, bS], [b_kT])
            proj_fm(win_d[l], O_MQK, 8, cons_qk)

            tg, b_tg = TG2, b_TG2

            def cons_mo(j, ps, bps):
                S, bS = sig3(ps, TT, bps)
                ts(tg[:, j, :], S, pp[:, l, P_MNW + j:P_MNW + j + 1], None, ALU.mult, None, [bS, b_pp], [b_tg])
            proj_fm(win_d[l], O_MO, 8, cons_mo)

            def cons_g(s, c, ps, bps):
                tt(sm[:, c, 0:8], ps, bp[:, l, B_BI:B_BI + 8], ALU.add, bps + [b_bp], [b_sm])
                act(sm[:, c, 12:16], sm[:, c, 4:8], AF.Exp, [b_sm], [b_sm], scale=-1.0)
                act(sm[:, c, 8:12], sm[:, c, 12:16], AF.Ln, [b_sm], [b_sm], bias=1.0)
            proj_tm(win_d[l], O_MI, 8, cons_g)

            def cons_v(s, c, ps, bps):
                act(VB2[:, c, s * 512:(s + 1) * 512], ps, AF.Copy, bps, [b_VB2])
            proj_tm(win_d[l], O_MV, 1024, cons_v)

        def mlstm_loop(l):
            qT, b_qT, kT, b_kT = qT2, b_qT2, kT2, b_kT2
            tg, b_tg = TG2, b_TG2
            vb, b_vb = VB2, b_VB2
            tk.dma("sp", mC[l][:].rearrange("p h v -> p (h v)"), mC_d[l], reads=[b_mCd[l]], writes=[b_mC[l]])
            act(mCb[:], mC[l][:], AF.Copy, [b_mC[l]], [b_mCb])
            act(mNb[:], mN[l][:], AF.Copy, [b_mN[l]], [b_mNb])
            yield
            for c in range(NCH):
                cs = slice(c * 128, (c + 1) * 128)
                ig = sm[:, c, 0:4]
                lfn = sm[:, c, 8:12]
                rhs1, b_rhs1 = F4[1], b_F4[1]
                tt(rhs1[:, 0:512].rearrange("p (h c) -> p h c", h=4), CST(C_TRI).unsqueeze(1).broadcast_to([128, 4, 128]),
                   lfn.unsqueeze(2).broadcast_to([128, 4, 128]), ALU.mult, [b_cst, b_sm], [b_rhs1])
                pL, bpL = pbankA()
                mm(pL[:, :], CST(C_UNEG), rhs1[:, 0:512], True, True, [b_cst, b_rhs1], bpL)
                yield
                Dt, b_Dt = F4[2], b_F4[2]
                for h in range(4):
                    act(Dt[:, h * 128:(h + 1) * 128], pL[:, h * 128:(h + 1) * 128], AF.Exp, bpL + [b_sm], [b_Dt],
                        bias=sm[:, c, h:h + 1])
                tt(Dt[:, 0:512].rearrange("p (h c) -> p h c", h=4), Dt[:, 0:512].rearrange("p (h c) -> p h c", h=4),
                   CST(C_MASKM).unsqueeze(1).broadcast_to([128, 4, 128]), ALU.mult, [b_Dt, b_cst], [b_Dt])
                yield
                pE, bpE = pbankA()
                mm(pE[:, :], CST(C_ONESNEG), rhs1[:, 0:512], True, True, [b_cst, b_rhs1], bpE)
                EB, b_EB = F4[3], b_F4[3]
                act(EB[:, 0:512], pE, AF.Exp, bpE, [b_EB])
                qtil, b_qtil = B2[1], b_B2[1]
                tt(qtil[:, 0:512].rearrange("p (h c) -> p h c", h=4), qT[:, :, cs], EB[:, 0:512].rearrange("p (h c) -> p h c", h=4),
                   ALU.mult, [b_qT, b_EB], [b_qtil])
                yield
                ps, bps = pbankA()
                for h in range(4):
                    mm(ps[:, h * 128:(h + 1) * 128], kT[:, h, cs], qT[:, h, cs], True, True, [b_kT, b_qT], bps)
                wT, b_wT = B2[0], b_B2[0]
                tt(wT[:, 0:512], ps, Dt[:, 0:512], ALU.mult, bps + [b_Dt], [b_wT])
                yield
                po, bpo = pbank2()
                pof = po.rearrange("p a b -> p (a b)")
                for h in range(4):
                    mm(pof[:, h * 256:(h + 1) * 256], wT[:, h * 128:(h + 1) * 128], vb[:, c, h * 256:(h + 1) * 256], True, False,
                       [b_wT, b_vb], bpo)
                    mm(pof[:, h * 256:(h + 1) * 256], qtil[:, h * 128:(h + 1) * 128], mCb[:, h, :], False, True, [b_qtil, b_mCb], bpo)
                pd, bpd = pbankA()
                for h in range(4):
                    mm(pd[:, h * 16:h * 16 + 1], wT[:, h * 128:(h + 1) * 128], onesb[:, 0:1], True, False, [b_wT, b_onesb], bpd)
                    mm(pd[:, h * 16:h * 16 + 1], qtil[:, h * 128:(h + 1) * 128], mNb[:, h:h + 1], False, True, [b_qtil, b_mNb], bpd)
                mm(pd[:, 64:68], CST(C_UNEG), lfn, True, True, [b_cst, b_sm], bpd)
                yield
                tt(st[:, 8:12], pd[:, 64:68], ig, ALU.add, bpd + [b_sm], [b_st])
                act(st[:, 8:12], st[:, 8:12], AF.Exp, [b_st], [b_st])
                yield
                vw, b_vw = B2[3], b_B2[3]
                tt(vw[:].rearrange("p (h v) -> p h v", h=4), vb[:, c, :].rearrange("p (h v) -> p h v", h=4),
                   st[:, 8:12].unsqueeze(2).broadcast_to([128, 4, 256]), ALU.mult, [b_vb, b_st], [b_vw])
                wstb, b_wstb = B2[4], b_B2[4]
                cp(wstb[:, 0:4], st[:, 8:12], [b_st], [b_wstb])
                pt, bpt = pbf()
                for h in range(4):
                    trp(pt[:, h * 128:(h + 1) * 128], kT[:, h, cs], [b_kT, b_idb], bpt)
                ktok, b_ktok = B2[5], b_B2[5]
                act(ktok[:, 0:512], pt[:, 0:512], AF.Identity, bpt, [b_ktok], scale=128.0 ** -0.5)
                yield
                den4 = pd[:, 0:64].rearrange("p (h s) -> p h s", s=16)[:, :, 0]
                ts(st[:, 20:24], den4, -1.0, 0.0, ALU.mult, ALU.add, bpd + [b_st], [b_st])
                tt(st[:, 12:16], den4, st[:, 20:24], ALU.max, bpd + [b_st], [b_st])
                ts(st[:, 12:16], st[:, 12:16], 1.0, 0.0, ALU.max, ALU.add, [b_st], [b_st])
                recip(st[:, 12:16], st[:, 12:16], [b_st], [b_st])
                act(F4[0][:], pof, AF.Square, bpo, [b_F4[0]])
                yield
                rsum(st[:, 0:4], F4[0][:].rearrange("p (h v) -> p h v", h=4), [b_F4[0]], [b_st])
                tt(st[:, 0:4], st[:, 0:4], st[:, 12:16], ALU.mult, [b_st], [b_st])
                tt(st[:, 0:4], st[:, 0:4], st[:, 12:16], ALU.mult, [b_st], [b_st])
                yield
                r = rstd_from_ss(st[:, 0:4], 4, 256, [])
                tt(st[:, 16:20], r, st[:, 12:16], ALU.mult, [b_st], [b_st])
                yield
                norm_transpose_out(pof, bpo, st[:, 16:20], 4, tg[:, :, cs], [b_tg], c, [b_st], YT2, b_YT2)
                yield
                pst, bpst = pbank2()
                pstf = pst.rearrange("p a b -> p (a b)")
                for h in range(4):
                    mm(pstf[:, h * 256:(h + 1) * 256], ktok[:, h * 128:(h + 1) * 128], vw[:, h * 256:(h + 1) * 256], True, True,
                       [b_ktok, b_vw], bpst)
                pn, bpn = pbankA()
                for h in range(4):
                    mm(pn[:, h * 16:h * 16 + 1], ktok[:, h * 128:(h + 1) * 128], wstb[:, h:h + 1], True, True, [b_ktok, b_wstb], bpn)
                yield
                EB3 = EB[:, 0:512].rearrange("p (h c) -> p h c", h=4)
                for h in range(4):
                    stt(mC[l][:, h, :], mC[l][:, h, :], EB[:, h * 128 + 127:h * 128 + 128], pstf[:, h * 256:(h + 1) * 256],
                        ALU.mult, ALU.add, [b_mC[l], b_EB] + bpst, [b_mC[l]])
                tt(mN[l][:], mN[l][:], EB3[:, :, 127], ALU.mult, [b_mN[l], b_EB], [b_mN[l]])
                tt(mN[l][:], mN[l][:], pn[:, 0:64].rearrange("p (h s) -> p h s", s=16)[:, :, 0], ALU.add, [b_mN[l]] + bpn, [b_mN[l]])
                act(mCb[:], mC[l][:], AF.Copy, [b_mC[l]], [b_mCb])
                act(mNb[:], mN[l][:], AF.Copy, [b_mN[l]], [b_mNb])
                yield
            tk.dma("sp", mC_d[l], mC[l][:].rearrange("p h v -> p (h v)"), reads=[b_mC[l]], writes=[b_mCd[l]])

        def ssd_proj(l):
            xT, b_xT = A8[0], b_A8[0]

            def cons_xbc(j, ps, bps):
                A, bA = conv(ps, bps, sh[l], b_sh[l], j, 4, P_SCW + j * 4, P_SCB + j, l)
                S, bS = sig3(A[:], TT, [bA])
                if j < 8:
                    tt(xT[:, j, :], A[:], S, ALU.mult, [bA, bS], [b_xT])
                elif j < 10:
                    tt(BT[:, j - 8, :], A[:], S, ALU.mult, [bA, bS], [b_BT])
                else:
                    tt(CT[:, j - 10, :], A[:], S, ALU.mult, [bA, bS], [b_CT])
            proj_fm(win_d[l], O_SXBC, 12, cons_xbc)

            act(aneg[:], bp[:, l, B_ALOG:B_ALOG + 16], AF.Exp, [b_bp], [b_aneg])
            ts(aneg[:], aneg[:], -1.0, None, ALU.mult, None, [b_aneg], [b_aneg])

            def cons_dt(s, c, ps, bps):
                tt(sm2[:, c, 48:64], ps, bp[:, l, B_DTB:B_DTB + 16], ALU.add, bps + [b_bp], [b_sm2])
                act(sm2[:, c, 48:64], sm2[:, c, 48:64], AF.Exp, [b_sm2], [b_sm2])
                act(sm2[:, c, 16:32], sm2[:, c, 48:64], AF.Ln, [b_sm2], [b_sm2], bias=1.0)
                tt(sm2[:, c, 32:48], sm2[:, c, 16:32], aneg[:], ALU.mult, [b_sm2, b_aneg], [b_sm2])
            proj_tm(win_d[l], O_SDT, 16, cons_dt)

            def cons_sz(s, c, ps, bps):
                S, bS = sig3(ps, 512, bps)
                tt(szs[:, c, s * 512:(s + 1) * 512], ps, S, ALU.mult, bps + [bS], [b_szs])
            proj_tm(win_d[l], O_SZ, 1024, cons_sz)

        def ssd_loop(l):
            xT, b_xT = A8[0], b_A8[0]
            tk.dma("sp", sS[l][:], sS_d[l], reads=[b_sSd[l]], writes=[b_sS[l]])
            act(sSb[:], sS[l][:], AF.Copy, [b_sS[l]], [b_sSb])
            yield
            for c in range(NCH):
                cs = slice(c * 128, (c + 1) * 128)
                dt = sm2[:, c, 16:32]
                dA = sm2[:, c, 32:48]
                pt, bpt = pbf()
                for j in range(8):
                    trp(pt[:, j * 128:(j + 1) * 128], xT[:, j, cs], [b_xT, b_idb], bpt)
                xtok, b_xtok = B2[0], b_B2[0]
                act(xtok[:], pt, AF.Copy, bpt, [b_xtok])
                pt2, bpt2 = pbf()
                for g in range(2):
                    trp(pt2[:, g * 128:(g + 1) * 128], BT[:, g, cs], [b_BT, b_idb], bpt2)
                btok, b_btok = B2[1], b_B2[1]
                act(btok[:, 0:256], pt2[:, 0:256], AF.Copy, bpt2, [b_btok])
                yield
                pd, bpd = pbankA()
                mm(pd[:, 0:16], CST(C_TRI), dA, True, True, [b_cst, b_sm2], bpd)
                mm(pd[:, 16:32], CST(C_UGT), dA, True, True, [b_cst, b_sm2], bpd)
                mm(pd[:, 32:48], CST(C_ONES), dA, True, True, [b_cst, b_sm2], bpd)
                act(st[:, 0:48], pd[:, 0:48], AF.Exp, bpd, [b_st])
                dec_s, b_dec_s = F4[1], b_F4[1]
                cp(dec_s[:, 0:48], st[:, 0:48], [b_st], [b_dec_s])
                eb = dec_s[:, 0:16]; erv = dec_s[:, 16:32]; EBL = dec_s[:, 32:48]
                xdt, b_xdt = B2[3], b_B2[3]
                tt(xdt[:].rearrange("p (h q) -> p h q", h=16), xtok[:].rearrange("p (h q) -> p h q", h=16),
                   dt.unsqueeze(2).broadcast_to([128, 16, 64]), ALU.mult, [b_xtok, b_sm2], [b_xdt])
                xw, b_xw = B2[4], b_B2[4]
                tt(xw[:].rearrange("p (h q) -> p h q", h=16), xdt[:].rearrange("p (h q) -> p h q", h=16),
                   erv.unsqueeze(2).broadcast_to([128, 16, 64]), ALU.mult, [b_xdt, b_dec_s], [b_xw])
                yield
                ps, bps = pbankA()
                for g in range(2):
                    mm(ps[:, g * 128:(g + 1) * 128], BT[:, g, cs], CT[:, g, cs], True, True, [b_BT, b_CT], bps)
                cbm, b_cbm = cbm_t, b_cbm_t
                tt(cbm[:, 0:256].rearrange("p (g c) -> p g c", g=2), ps[:, 0:256].rearrange("p (g c) -> p g c", g=2),
                   CST(C_TRI).unsqueeze(1).broadcast_to([128, 2, 128]), ALU.mult, bps + [b_cst], [b_cbm])
                for g in range(2):
                    yield
                    rhs1, b_rhs1 = F4[2], b_F4[2]
                    tt(rhs1[:].rearrange("p (h c) -> p h c", h=8), CST(C_TRI).unsqueeze(1).broadcast_to([128, 8, 128]),
                       sm2[:, c, 32 + g * 8:32 + (g + 1) * 8].unsqueeze(2).broadcast_to([128, 8, 128]), ALU.mult,
                       [b_cst, b_sm2], [b_rhs1])
                    pL, bpL = pbank2()
                    for hh in range(2):
                        mm(pL[:, hh, :], CST(C_UGT), rhs1[:, hh * 512:(hh + 1) * 512], True, True, [b_cst, b_rhs1], bpL)
                    dec, b_dec = F4[3], b_F4[3]
                    act(dec[:], pL.rearrange("p a b -> p (a b)"), AF.Exp, bpL, [b_dec])
                    tt(mixT[:, g * 8:(g + 1) * 8, :], dec[:].rearrange("p (h c) -> p h c", h=8),
                       cbm[:, g * 128:(g + 1) * 128].unsqueeze(1).broadcast_to([128, 8, 128]), ALU.mult, [b_dec, b_cbm], [b_mixT])
                yield
                pi, bpi = pbank2()
                for g in range(2):
                    mm(pi[:, g, :], CT[:, g, cs], sSb[:, g * 512:(g + 1) * 512], True, True, [b_CT, b_sSb], bpi)
                t1, b_t1 = F4[0], b_F4[0]
                tt(t1[:].rearrange("p (h q) -> p h q", h=16), pi.rearrange("p a (h q) -> p (a h) q", q=64),
                   eb.unsqueeze(2).broadcast_to([128, 16, 64]), ALU.mult, bpi + [b_dec_s], [b_t1])
                yield
                py, bpy = pbank2()
                pyf = py.rearrange("p a b -> p (a b)")
                for h in range(16):
                    mm(pyf[:, h * 64:(h + 1) * 64], mixT[:, h, :], xdt[:, h * 64:(h + 1) * 64], True, True, [b_mixT, b_xdt], bpy)
                tt(t1[:], t1[:], pyf, ALU.add, [b_t1] + bpy, [b_t1])
                yield
                pst, bpst = pbank2()
                for g in range(2):
                    mm(pst[:, g, :], btok[:, g * 128:(g + 1) * 128], xw[:, g * 512:(g + 1) * 512], True, True, [b_btok, b_xw], bpst)
                tt(sS[l][:].rearrange("p (h q) -> p h q", h=16), sS[l][:].rearrange("p (h q) -> p h q", h=16),
                   EBL.unsqueeze(2).broadcast_to([128, 16, 64]), ALU.mult, [b_sS[l], b_dec_s], [b_sS[l]])
                tt(sS[l][:], sS[l][:], pst.rearrange("p a b -> p (a b)"), ALU.add, [b_sS[l]] + bpst, [b_sS[l]])
                act(sSb[:], sS[l][:], AF.Copy, [b_sS[l]], [b_sSb])
                yield
                t2, b_t2 = F4[2], b_F4[2]
                tt(t2[:].rearrange("p (h q) -> p h q", h=16), xtok[:].rearrange("p (h q) -> p h q", h=16),
                   bp[:, l, B_SD:B_SD + 16].unsqueeze(2).broadcast_to([128, 16, 64]), ALU.mult, [b_xtok, b_bp], [b_t2])
                tt(t1[:], t1[:], t2[:], ALU.add, [b_t1, b_t2], [b_t1])
                tt(t1[:], t1[:], szs[:, c, :], ALU.mult, [b_t1, b_szs], [b_t1])
                yield
                act(F4[3][:], t1[:], AF.Square, [b_t1], [b_F4[3]])
                rsum(st[:, 0:2], F4[3][:].rearrange("p (h v) -> p h v", h=2), [b_F4[3]], [b_st])
                yield
                r = rstd_from_ss(st[:, 0:2], 2, 512, [])
                yield
                norm_transpose_out(t1[:], [b_t1], r, 2, pp[:, l, P_SNW:P_SNW + 8].unsqueeze(2).broadcast_to([128, 8, 128]),
                                   [b_pp], c, [b_st], A8[2], b_A8[2])
                yield
            tk.dma("sp", sS_d[l], sS[l][:], reads=[b_sS[l]], writes=[b_sSd[l]])

        def outproj(l):
            mb, b_mb = A8[1], b_A8[1]
            act(mb[:], merged[:], AF.Copy, [b_merged], [b_mb])
            for half in range(2):
                sl, bs = slab(wout_d[l][:, half * 512:(half + 1) * 512], 8, 512)
                for c in range(NCH):
                    ps, bps = pbank()
                    for kd in range(8):
                        mm(ps[:, :], mb[:, kd, c * 128:(c + 1) * 128], sl[:, kd, :], kd == 0, kd == 7, [b_mb, bs], bps)
                    tt(hres[:, c, half * 512:(half + 1) * 512], hres[:, c, half * 512:(half + 1) * 512], ps, ALU.add,
                       [b_h] + bps, [b_h])

        def ffn_phase(l):
            rmsnorm_to_uT(l, P_NFFN)
            hid = A8
            j = 0
            while j < 22:
                n = min(4, 22 - j)
                slg, bsg = slab(wup_d[l][:, j * 128:(j + n) * 128], 8, n * 128)
                slv, bsv = slab(wup_d[l][:, DFF + j * 128:DFF + (j + n) * 128], 8, n * 128)
                for jj in range(n):
                    J = j + jj
                    ps, bps = pbank()
                    for kd in range(8):
                        mm(ps[:, 0:TT], slg[:, kd, jj * 128:(jj + 1) * 128], uT[:, kd, :], kd == 0, kd == 7, [bsg, b_uT], bps)
                    Ag, bAg = conv(ps[:, 0:TT], bps, fh[l], b_fh[l], J, 3, P_FCW + J * 3, P_FCB + J, l)
                    S, bS = sig3(Ag[:], TT, [bAg])
                    tt(Ag[:], Ag[:], S, ALU.mult, [bAg, bS], [bAg])
                    ps2, bps2 = pbank()
                    for kd in range(8):
                        mm(ps2[:, 0:TT], slv[:, kd, jj * 128:(jj + 1) * 128], uT[:, kd, :], kd == 0, kd == 7, [bsv, b_uT], bps2)
                    Av, bAv = conv(ps2[:, 0:TT], bps2, fh[l], b_fh[l], 22 + J, 3, P_FCW + (22 + J) * 3, P_FCB + 22 + J, l)
                    tt(hid[J // 8][:, J % 8, :], Ag[:], Av[:], ALU.mult, [bAg, bAv], [b_A8[J // 8]])
                j += n
            for half in range(2):
                pss = [pbank() for _ in range(NCH)]
                for kg in range(3):
                    nk = 8 if kg < 2 else 6
                    sl, bs = slab(wdn_d[l][kg * 1024:kg * 1024 + nk * 128, half * 512:(half + 1) * 512], nk, 512)
                    for c in range(NCH):
                        ps, bps = pss[c]
                        for kk in range(nk):
                            J = kg * 8 + kk
                            mm(ps[:, :], hid[J // 8][:, J % 8, c * 128:(c + 1) * 128], sl[:, kk, :], J == 0, J == 21,
                               [b_A8[J // 8], bs], bps)
                for c in range(NCH):
                    ps, bps = pss[c]
                    tt(hres[:, c, half * 512:(half + 1) * 512], hres[:, c, half * 512:(half + 1) * 512], ps, ALU.add,
                       [b_h] + bps, [b_h])

        def final_norm_store(t):
            nf, b_nf = F4[3], b_F4[3]
            tk.dma("sp", nf[:], nf_d, writes=[b_nf])
            for c in range(NCH):
                tk.op("dve", lambda e: e.memset(st[:, 0:1], 0.0), writes=[b_st])
                act(F4[0][:], hres[:, c, :], AF.Square, [b_h], [b_F4[0], b_st], accum=st[:, 0:1])
                r = rstd_from_ss(st[:, 0:1], 1, D, [])
                o, b_o = F4[1 + (c % 2)], b_F4[1 + (c % 2)]
                stt(o[:], hres[:, c, :], r, nf[:], ALU.mult, ALU.mult, [b_h, b_st, b_nf], [b_o])
                r0 = t * TT + c * 128
                tk.dma("sp", out_d[r0:r0 + 128, :], o[:], reads=[b_o])

        def emit():
            rr.update({"p1": 0, "a1": 0, "pb": 0, "xs": 0, "acc": 0, "sg": 0})
            tk.dma("sp", pp[:], pp_d.rearrange("l p n -> p l n"), writes=[b_pp])
            tk.dma("sp", bp[:], bp_d.rearrange("l p n -> p l n"), writes=[b_bp])
            tk.dma("sp", cst[:], cst_d.rearrange("p (k n) -> p k n", k=NCONST), writes=[b_cst])
            tk.dma("sp", idf[:], idn_d, writes=[b_idf])
            cp(idb[:], idf[:], [b_idf], [b_idb])
            ts(ngb[:], pp[:, :, P_GB:P_GB + 24], -1.0, None, ALU.mult, None, [b_pp], [b_ngb])
            tk.op("dve", lambda e: e.memset(onesb[:], 1.0), writes=[b_onesb])
            tk.op("dve", lambda e: e.memset(gaT[:], 1.0), writes=[b_gaT])
            tk.op("dve", lambda e: e.memset(wab[:], 0.0), writes=[b_wab])
            tk.op("dve", lambda e: e.memset(F4[0][:], 0.0), writes=[b_F4[0]])
            for l in range(NL):
                for t_, b_ in ((mN[l], b_mN[l]), (mh[l], b_mh[l]), (sh[l], b_sh[l]), (fh[l], b_fh[l])):
                    tk.op("dve", lambda e: e.memset(t_[:], 0.0), writes=[b_])
                tk.dma("sp", gS_d[l], F4[0][:], reads=[b_F4[0]], writes=[b_gSd[l]])
                tk.dma("sp", mC_d[l], F4[0][:], reads=[b_F4[0]], writes=[b_mCd[l]])
                tk.dma("sp", sS_d[l], F4[0][:], reads=[b_F4[0]], writes=[b_sSd[l]])
            for t in range(NTILES):
                tk.dma("sp", hres[:], x_d[t * TT:(t + 1) * TT, :].rearrange("(c p) d -> p c d", p=128), writes=[b_h])
                for l in range(NL):
                    rmsnorm_to_uT(l, P_NMIX)
                    gla_proj(l)
                    bg_run(gla_loop(l), lambda: mlstm_proj(l))
                    bg_run(mlstm_loop(l), lambda: (branch(l, 0, A8[2], b_A8[2]), ssd_proj(l)))
                    bg_run(ssd_loop(l), lambda: branch(l, 1, YT2, b_YT2))
                    branch(l, 2, A8[2], b_A8[2])
                    outproj(l)
                    ffn_phase(l)
                final_norm_store(t)
            if not tk.dry:
                for i in range(len(tk.dsem)):
                    if tk.dcnt[i]:
                        tk._wait("sp", (i, tk.dcnt[i]))

        tk.dry = True
        emit()
        tk.dry = False
        emit()
        _bp.last_nins = tk.nins
    return nc


build_program = _bp
build_program.debug = False


def _consts():
    k = np.arange(128)[:, None]
    c = np.arange(128)[None, :]
    tri = (k <= c).astype(np.float32)
    ugt = (k > c).astype(np.float32)
    ones = np.ones((128, 128), np.float32)
    lst = [tri, ugt, tri * (-1.0 / 16.0), ugt * (-1.0 / 16.0), -ugt, ones, -ones, tri * (128.0 ** -0.5)]
    return np.ascontiguousarray(np.concatenate(lst, axis=1).astype(np.float32))


def _fm(v):
    v = np.asarray(v, np.float32)
    return v.reshape(-1, 128).T


def _pack_params(inp, NL):
    pp = np.zeros((NL, 128, NPP), np.float32)
    bp = np.zeros((NL, 128, NBP), np.float32)
    for l in range(NL):
        pp[l, :, P_NMIX:P_NMIX + 8] = _fm(inp["norm_mix"][l])
        pp[l, :, P_NFFN:P_NFFN + 8] = _fm(inp["norm_ffn"][l])
        pp[l, :, P_GNW:P_GNW + 8] = _fm(inp["gla_norm"][l])
        pp[l, :, P_MNW:P_MNW + 8] = _fm(inp["mlstm_norm"][l])
        pp[l, :, P_SNW:P_SNW + 8] = _fm(inp["ssd_norm"][l])
        w = np.asarray(inp["mlstm_conv_w"][l])
        pp[l, :, P_MCW:P_MCW + 32] = np.stack([_fm(w[k]) for k in range(4)], axis=2).reshape(128, 32)
        pp[l, :, P_MCB:P_MCB + 8] = _fm(inp["mlstm_conv_b"][l])
        w = np.asarray(inp["ssd_conv_w"][l])
        pp[l, :, P_SCW:P_SCW + 48] = np.stack([_fm(w[k]) for k in range(4)], axis=2).reshape(128, 48)
        pp[l, :, P_SCB:P_SCB + 12] = _fm(inp["ssd_conv_b"][l])
        w = np.asarray(inp["ffn_conv_w"][l])
        pp[l, :, P_FCW:P_FCW + 132] = np.stack([_fm(w[k]) for k in range(3)], axis=2).reshape(128, 132)
        pp[l, :, P_FCB:P_FCB + 44] = _fm(inp["ffn_conv_b"][l])
        pp[l, :, P_GB:P_GB + 24] = _fm(inp["gate_b"][l])
        row = np.concatenate([inp["mlstm_bi"][l], inp["mlstm_bf"][l], inp["ssd_dt_bias"][l], inp["ssd_a_log"][l],
                              inp["ssd_d"][l]]).astype(np.float32)
        bp[l] = np.broadcast_to(row[None, :], (128, NBP))
    return pp, bp


_CACHE = {}


def run(inputs, NL, B, T, NCH=2, n_cores=None):
    inp = {k: np.asarray(v) for k, v in inputs.items()}
    TT = NCH * 128
    NT = T // TT
    key = (NL, NT, NCH)
    if key not in _CACHE:
        _CACHE[key] = build_program(NL, NT, NCH)
    nc = _CACHE[key]
    pp, bp = _pack_params(inp, NL)
    wab = np.ascontiguousarray(np.concatenate([inp["gla_wa"][:NL], inp["gla_ba"][:NL, None, :]], axis=1).astype(np.float32))
    nf = np.ascontiguousarray(np.broadcast_to(inp["norm_final"].astype(np.float32)[None, :], (128, D)))
    shared = dict(
        w_in=np.ascontiguousarray(inp["w_in"][:NL], np.float32),
        w_branch=np.ascontiguousarray(inp["w_branch"][:NL], np.float32),
        w_out=np.ascontiguousarray(inp["w_out"][:NL], np.float32),
        w_up=np.ascontiguousarray(inp["w_up"][:NL], np.float32),
        w_down=np.ascontiguousarray(inp["w_down"][:NL], np.float32),
        gla_wab=wab, pp=pp, bp=bp, nf=nf, consts=_consts(), ident=np.eye(128, dtype=np.float32),
    )
    in_maps = []
    for b in range(B):
        m = dict(shared)
        m["x"] = np.ascontiguousarray(inp["x"][b, :T], np.float32)
        in_maps.append(m)
    res = run_bass_kernel_spmd(nc, in_maps, core_ids=list(range(B)))
    if _bp.debug:
        run.dbg = np.asarray(res.results[0]["dbg"])
    return np.stack([np.asarray(r["out"]) for r in res.results], axis=0).astype(np.float32)


def kernel(**inputs):
    return run(inputs, NL=4, B=4, T=4096, NCH=2)
```

```python
import numpy as np
from collections import deque
from contextlib import ExitStack
import concourse.bass as bass
import concourse.mybir as mybir
from concourse.bass_utils import run_bass_kernel_spmd

F32 = mybir.dt.float32
BF16 = mybir.dt.bfloat16
AF = mybir.ActivationFunctionType
ALU = mybir.AluOpType
AX = mybir.AxisListType

D = 1024
DIN = 11816
DFF = 2816
EPS = 1e-6
O_GQ, O_GK, O_GV, O_GA, O_GG = 0, 512, 1024, 2048, 2064
O_MQK, O_MV, O_MI, O_MO = 3088, 4112, 5136, 5144
O_SZ, O_SXBC, O_SDT, O_GATES = 6168, 7192, 8728, 8744
P_NMIX, P_NFFN, P_GNW, P_MNW, P_SNW, P_MCW, P_MCB, P_SCW, P_SCB, P_FCW, P_FCB, P_GB, NPP = \
    0, 8, 16, 24, 32, 40, 72, 80, 128, 140, 272, 316, 340
B_BI, B_DTB, B_ALOG, B_SD, NBP = 0, 8, 24, 40, 56
C_TRI, C_UGT, C_TRIG, C_UG, C_UNEG, C_ONES, C_ONESNEG, C_MASKM, NCONST = 0, 1, 2, 3, 4, 5, 6, 7, 8


class Buf:
    __slots__ = ("lw", "rd")

    def __init__(self):
        self.lw = None
        self.rd = []


class TK:
    def __init__(self, nc, es, n_dma_sems=12):
        self.nc = nc
        self.eng = {"pe": nc.tensor, "act": nc.scalar, "dve": nc.vector, "pool": nc.gpsimd, "sp": nc.sync}
        self.sem = {}
        self.cnt = {}
        for e in ["pe", "act", "dve", "pool"]:
            self.sem[e] = es.enter_context(nc.semaphore("s_" + e))
            self.cnt[e] = 0
        self.dsem = [es.enter_context(nc.semaphore("d%d" % i)) for i in range(n_dma_sems)]
        self.dcnt = [0] * n_dma_sems
        self.dnext = 0
        self.waited = {}
        self.dry = False
        self.nins = 0
        self.dry_count = 0
        self.defer_q = None
        self.pending = None
        self.pop_every = 1
        self.b_count = 0

    def _wait(self, e, dep):
        key, val = dep
        if key == e and e == "pe":
            return
        w = self.waited.setdefault(e, {})
        if w.get(key, 0) >= val:
            return
        w[key] = val
        sem = self.sem[key] if isinstance(key, str) else self.dsem[key]
        self.eng[e].wait_ge(sem, val)
        self.nins += 1

    def _deps(self, e, reads, writes):
        for b in reads:
            if b.lw is not None:
                self._wait(e, b.lw)
        for b in writes:
            if b.lw is not None:
                self._wait(e, b.lw)
            for r in b.rd:
                self._wait(e, r)

    def _mark(self, tag, reads, writes):
        for b in reads:
            b.rd.append(tag)
            if len(b.rd) > 64:
                last = {}
                for k, v in b.rd:
                    last[k] = max(last.get(k, 0), v)
                b.rd = list(last.items())
        for b in writes:
            b.lw = tag
            b.rd = []

    def _tick(self):
        if self.pending:
            self.b_count += 1
            if self.b_count >= self.pop_every:
                self.b_count = 0
                self.pending.popleft()()

    def op(self, e, fn, reads=(), writes=()):
        if self.dry:
            self.dry_count += 1
            return
        if self.defer_q is not None:
            self.defer_q.append(lambda: self._op(e, fn, reads, writes))
            return
        self._op(e, fn, reads, writes)
        self._tick()

    def dma(self, e, out, in_, reads=(), writes=()):
        if self.dry:
            self.dry_count += 1
            return
        if self.defer_q is not None:
            self.defer_q.append(lambda: self._dma(e, out, in_, reads, writes))
            return
        self._dma(e, out, in_, reads, writes)
        self._tick()

    def _op(self, e, fn, reads=(), writes=()):
        self._deps(e, reads, writes)
        ins = fn(self.eng[e])
        self.cnt[e] += 1
        self.nins += 1
        ins.then_inc(self.sem[e], 1)
        self._mark((e, self.cnt[e]), reads, writes)

    def _dma(self, e, out, in_, reads=(), writes=()):
        i = self.dnext
        self.dnext = (self.dnext + 1) % len(self.dsem)
        if self.dcnt[i] > 0:
            self._wait(e, (i, self.dcnt[i]))
        self._deps(e, reads, writes)
        ins = self.eng[e].dma_start(out=out, in_=in_)
        self.nins += 1
        self.dcnt[i] += 16
        ins.then_inc(self.dsem[i], 16)
        self._mark((i, self.dcnt[i]), reads, writes)


def build_program(NL, NTILES, NCH=2, NSLAB=3, LOOK=2):
    pass


def _bp(NL, NTILES, NCH=2, NSLAB=5, LOOK=3):
    TT = NCH * 128
    T = NTILES * TT
    nc = bass.Bass("TRN2", target_bir_lowering=False)
    dr = lambda name, shape, kind="ExternalInput": nc.dram_tensor(name, shape, F32, kind=kind).ap()
    x_d = dr("x", [T, D])
    win_d = dr("w_in", [NL, D, DIN])
    wbr_d = dr("w_branch", [NL, 3, D, D])
    wout_d = dr("w_out", [NL, D, D])
    wup_d = dr("w_up", [NL, D, 2 * DFF])
    wdn_d = dr("w_down", [NL, DFF, D])
    wab_d = dr("gla_wab", [NL, 17, 512])
    pp_d = dr("pp", [NL, 128, NPP])
    bp_d = dr("bp", [NL, 128, NBP])
    nf_d = dr("nf", [128, D])
    cst_d = dr("consts", [128, NCONST * 128])
    idn_d = dr("ident", [128, 128])
    out_d = dr("out", [T, D], kind="ExternalOutput")
    DBG = _bp.debug
    dbg_d = dr("dbg", [24, 128, 2048], kind="ExternalOutput") if DBG else None

    with ExitStack() as es:
        tk = TK(nc, es)
        _n = [0]

        def sb(shape, dt):
            _n[0] += 1
            return es.enter_context(nc.sbuf_tensor("t%d" % _n[0], shape, dt))

        hres = sb([128, NCH, D], F32); b_h = Buf()
        uT = sb([128, 8, TT], BF16); b_uT = Buf()
        xn = sb([128, D], BF16); b_xn = Buf()
        xnn = [xn, sb([128, D], BF16)]; b_xnn = [b_xn, Buf()]
        stn = [sb([128, 8], F32) for _ in range(NCH)]; b_stn = [Buf() for _ in range(NCH)]
        _gS1 = sb([128, 4, 256], F32); _bgS1 = Buf()
        _mC1 = sb([128, 4, 256], F32); _bmC1 = Buf()
        _sS1 = sb([128, 1024], F32); _bsS1 = Buf()
        gS = [_gS1] * NL; b_gS = [_bgS1] * NL
        mC = [_mC1] * NL; b_mC = [_bmC1] * NL
        gS_d = nc.dram_tensor("gS_d", [NL, 128, 1024], F32).ap(); b_gSd = [Buf() for _ in range(NL)]
        mC_d = nc.dram_tensor("mC_d", [NL, 128, 1024], F32).ap(); b_mCd = [Buf() for _ in range(NL)]
        sS_d = nc.dram_tensor("sS_d", [NL, 128, 1024], F32).ap(); b_sSd = [Buf() for _ in range(NL)]
        mN = [sb([128, 4], F32) for _ in range(NL)]; b_mN = [Buf() for _ in range(NL)]
        sS = [_sS1] * NL; b_sS = [_bsS1] * NL
        gSb = sb([128, 4, 256], BF16); b_gSb = Buf()
        mCb = sb([128, 4, 256], BF16); b_mCb = Buf()
        mNb = sb([128, 4], BF16); b_mNb = Buf()
        sSb = sb([128, 1024], BF16); b_sSb = Buf()
        mh = [sb([128, 8, 3], F32) for _ in range(NL)]; b_mh = [Buf() for _ in range(NL)]
        sh = [sb([128, 12, 3], F32) for _ in range(NL)]; b_sh = [Buf() for _ in range(NL)]
        fh = [sb([128, 44, 2], F32) for _ in range(NL)]; b_fh = [Buf() for _ in range(NL)]
        pp = sb([128, NL, NPP], F32); b_pp = Buf()
        bp = sb([128, NL, NBP], F32); b_bp = Buf()
        cst = sb([128, NCONST, 128], F32); b_cst = Buf()
        idf = sb([128, 128], F32); b_idf = Buf()
        idb = sb([128, 128], BF16); b_idb = Buf()
        onesb = sb([128, 4], BF16); b_onesb = Buf()
        slabs = [sb([128, 8, 512], BF16) for _ in range(NSLAB)]; b_slab = [Buf() for _ in range(NSLAB)]
        wab = sb([32, 512], F32); b_wab = Buf()
        gaT = sb([32, TT], F32); b_gaT = Buf()
        aneg = sb([128, 16], F32); b_aneg = Buf()
        A8 = [sb([128, 8, TT], BF16) for _ in range(3)]; b_A8 = [Buf() for _ in range(3)]
        qdT = sb([128, 4, TT], BF16); b_qdT = Buf()
        kiT = sb([128, 4, TT], BF16); b_kiT = Buf()
        qT2 = sb([128, 4, TT], BF16); b_qT2 = Buf()
        kT2 = sb([128, 4, TT], BF16); b_kT2 = Buf()
        TG2 = sb([128, 8, TT], BF16); b_TG2 = Buf()
        YT2 = sb([128, 8, TT], BF16); b_YT2 = Buf()
        VB2 = sb([128, NCH, 1024], BF16); b_VB2 = Buf()
        cbm_t = sb([128, 256], F32); b_cbm_t = Buf()
        sm2 = sb([128, NCH, 64], F32); b_sm2 = Buf()
        la = sb([128, NCH, 512], F32); b_la = Buf()
        Epos = sb([128, 4, TT], F32); b_Epos = Buf()
        Eneg = sb([128, 4, TT], F32); b_Eneg = Buf()
        merged = sb([128, 8, TT], F32); b_merged = Buf()
        vb = sb([128, NCH, 1024], BF16); b_vb = Buf()
        kdec = sb([128, NCH, 512], BF16); b_kdec = Buf()
        erev = sb([128, NCH, 512], F32); b_erev = Buf()
        BT = sb([128, 2, TT], BF16); b_BT = Buf()
        CT = sb([128, 2, TT], BF16); b_CT = Buf()
        szs = sb([128, NCH, 1024], BF16); b_szs = Buf()
        sm = sb([128, NCH, 64], F32); b_sm = Buf()
        st = sb([128, 64], F32); b_st = Buf()
        F4 = [sb([128, 1024], F32) for _ in range(4)]; b_F4 = [Buf() for _ in range(4)]
        B2 = [sb([128, 1024], BF16) for _ in range(6)]; b_B2 = [Buf() for _ in range(6)]
        mixT = sb([128, 16, 128], BF16); b_mixT = Buf()
        xs = [sb([128, TT + 4], F32) for _ in range(2)]; b_xs = [Buf() for _ in range(2)]
        acc = [sb([128, TT], F32) for _ in range(3)]; b_acc = [Buf() for _ in range(3)]
        psf = es.enter_context(nc.psum_tensor("psf", [128, 6, 512], F32)); b_psf = [Buf() for _ in range(6)]
        psb = es.enter_context(nc.psum_tensor("psb", [128, 2, 1024], BF16)); b_psb = [Buf() for _ in range(2)]
        rr = {"p1": 0, "a1": 0, "pb": 0, "xs": 0, "acc": 0}

        def pbank():
            i = rr["p1"]; rr["p1"] = (i + 1) % 2
            return psf[:, i, :], [b_psf[i]]

        def pbankA():
            i = rr["a1"]; rr["a1"] = (i + 1) % 2
            return psf[:, 2 + i, :], [b_psf[2 + i]]

        def pbank2():
            return psf[:, 4:6, :], [b_psf[4], b_psf[5]]

        bg = [None]

        def bg_step():
            g = bg[0]
            if g is not None:
                try:
                    next(g)
                except StopIteration:
                    bg[0] = None

        bg_counts = []
        bg_idx = [0]

        def bg_run(gen, fn):
            if tk.dry:
                c0 = tk.dry_count
                for _ in gen:
                    pass
                nA = tk.dry_count - c0
                c0 = tk.dry_count
                fn()
                bg_counts.append((nA, tk.dry_count - c0))
                return
            nA, nB = bg_counts[bg_idx[0]]
            bg_idx[0] += 1
            tk.defer_q = []
            for _ in gen:
                pass
            q = tk.defer_q
            tk.defer_q = None
            tk.pending = deque(q)
            tk.pop_every = 1
            tk.b_count = 0
            fn()
            while tk.pending:
                tk.pending.popleft()()
            tk.pending = None

        def pbf():
            i = rr["pb"]; rr["pb"] = (i + 1) % 2
            return psb[:, i, :], [b_psb[i]]

        def CST(i):
            return cst[:, i, :]

        dbgbuf = sb([128, 2048], F32) if DBG else None
        b_dbg = Buf()

        def dump(idx, ap, rbufs, n):
            if not DBG:
                return
            cp(dbgbuf[:, 0:n], ap, rbufs, [b_dbg])
            tk.dma("sp", dbg_d[idx, :, 0:n], dbgbuf[:, 0:n], reads=[b_dbg])

        slab_specs = []
        slab_state = {"next": 0, "issued": 0}

        def issue_slab(i):
            src, nk, n = slab_specs[i]
            bi = i % NSLAB
            tk.dma("pool", slabs[bi][:, 0:nk, 0:n], src.rearrange("(k p) n -> p k n", p=128), writes=[b_slab[bi]])

        def slab(src, nk, n):
            if tk.dry:
                slab_specs.append((src, nk, n))
                return slabs[0], b_slab[0]
            i = slab_state["next"]; slab_state["next"] += 1
            lim = min(i + LOOK, len(slab_specs) - 1)
            while slab_state["issued"] <= lim:
                issue_slab(slab_state["issued"]); slab_state["issued"] += 1
            return slabs[i % NSLAB], b_slab[i % NSLAB]

        def mm(out, lhsT, rhs, start, stop, reads, writes):
            tk.op("pe", lambda e: e.matmul(out, lhsT=lhsT, rhs=rhs, start=start, stop=stop), reads=reads, writes=writes)

        def act(out, in_, func, reads, writes, scale=1.0, bias=None, accum=None):
            kw = {}
            if bias is not None:
                kw["bias"] = bias
            if accum is not None:
                kw["accum_out"] = accum
            tk.op("act", lambda e: e.activation(out=out, in_=in_, func=func, scale=scale, **kw), reads=reads, writes=writes)

        def tt(out, in0, in1, op, reads, writes):
            tk.op("dve", lambda e: e.tensor_tensor(out=out, in0=in0, in1=in1, op=op), reads=reads, writes=writes)

        def stt(out, in0, scalar, in1, op0, op1, reads, writes):
            tk.op("dve", lambda e: e.scalar_tensor_tensor(out=out, in0=in0, scalar=scalar, in1=in1, op0=op0, op1=op1),
                  reads=reads, writes=writes)

        def ts(out, in0, s1, s2, op0, op1, reads, writes):
            if s2 is None:
                tk.op("dve", lambda e: e.tensor_scalar(out=out, in0=in0, scalar1=s1, scalar2=0.0, op0=op0, op1=ALU.add), reads=reads, writes=writes)
            else:
                tk.op("dve", lambda e: e.tensor_scalar(out=out, in0=in0, scalar1=s1, scalar2=s2, op0=op0, op1=op1),
                      reads=reads, writes=writes)

        def trp(out, in_, reads, writes):
            tk.op("pe", lambda e: e.transpose(out=out, in_=in_, identity=idb[:]), reads=reads, writes=writes)

        def rsum(out, in_, reads, writes):
            tk.op("dve", lambda e: e.reduce_sum(out=out, in_=in_, axis=AX.X), reads=reads, writes=writes)

        def recip(out, in_, reads, writes):
            tk.op("dve", lambda e: e.reciprocal(out=out, in_=in_), reads=reads, writes=writes)

        def cp(out, in_, reads, writes):
            tk.op("dve", lambda e: e.tensor_copy(out=out, in_=in_), reads=reads, writes=writes)

        def proj_fm(wsrc, col0, nchunks, consume, rhsT=None, rb=None):
            rhsT = uT if rhsT is None else rhsT
            rb = b_uT if rb is None else rb
            j = 0
            while j < nchunks:
                n = min(4, nchunks - j)
                sl, bs = slab(wsrc[:, col0 + j * 128: col0 + (j + n) * 128], 8, n * 128)
                for jj in range(n):
                    ps, bps = pbank()
                    for kd in range(8):
                        mm(ps[:, 0:TT], sl[:, kd, jj * 128:(jj + 1) * 128], rhsT[:, kd, :], kd == 0, kd == 7,
                           [bs, rb], bps)
                    consume(j + jj, ps[:, 0:TT], bps)
                    bg_step()
                j += n

        def proj_tm(wsrc, col0, ncols, consume):
            s = 0
            while s * 512 < ncols:
                n = min(512, ncols - s * 512)
                sl, bs = slab(wsrc[:, col0 + s * 512: col0 + s * 512 + n], 8, n)
                for c in range(NCH):
                    ps, bps = pbank()
                    for kd in range(8):
                        mm(ps[:, 0:n], uT[:, kd, c * 128:(c + 1) * 128], sl[:, kd, 0:n], kd == 0, kd == 7, [bs, b_uT], bps)
                    consume(s, c, ps[:, 0:n], bps)
                    bg_step()
                s += 1

        def rstd_from_ss(ss_ap, n, width, reads):
            o = st[:, 32:32 + n]
            ts(o, ss_ap, 1.0 / width, EPS, ALU.mult, ALU.add, reads + [b_st], [b_st])
            act(o, o, AF.Ln, [b_st], [b_st])
            act(o, o, AF.Exp, [b_st], [b_st], scale=-0.5)
            return o

        def rmsnorm_to_uT(l, pcol):
            pts = []
            for c in range(NCH):
                tk.op("dve", lambda e: e.memset(stn[c][:, 0:1], 0.0), writes=[b_stn[c]])
            for c in range(NCH):
                act(F4[c % 4][:], hres[:, c, :], AF.Square, [b_h], [b_F4[c % 4], b_stn[c]], accum=stn[c][:, 0:1])
            for c in range(NCH):
                ts(stn[c][:, 1:2], stn[c][:, 0:1], 1.0 / D, EPS, ALU.mult, ALU.add, [b_stn[c]], [b_stn[c]])
            for c in range(NCH):
                act(stn[c][:, 1:2], stn[c][:, 1:2], AF.Ln, [b_stn[c]], [b_stn[c]])
            for c in range(NCH):
                act(stn[c][:, 1:2], stn[c][:, 1:2], AF.Exp, [b_stn[c]], [b_stn[c]], scale=-0.5)
            for c in range(NCH):
                act(xnn[c % 2][:], hres[:, c, :], AF.Identity, [b_h, b_stn[c]], [b_xnn[c % 2]], scale=stn[c][:, 1:2])
                pt, bpt = pbf()
                pts.append((pt, bpt))
                for kd in range(8):
                    trp(pt[:, kd * 128:(kd + 1) * 128], xnn[c % 2][:, kd * 128:(kd + 1) * 128], [b_xnn[c % 2], b_idb], bpt)
            for c in range(NCH):
                pt, bpt = pts[c]
                tt(uT[:, :, c * 128:(c + 1) * 128], pt.rearrange("p (k t) -> p k t", k=8),
                   pp[:, l, pcol:pcol + 8].unsqueeze(2).broadcast_to([128, 8, 128]), ALU.mult, bpt + [b_pp], [b_uT])

        def conv(ps, bps, hal, b_hal, j, K, wbase, bcol, l):
            i = rr["xs"]; rr["xs"] = (i + 1) % 2
            a = rr["acc"]; rr["acc"] = (a + 1) % 3
            X, bX = xs[i], b_xs[i]
            A, bA = acc[a], b_acc[a]
            act(X[:, K - 1:K - 1 + TT], ps, AF.Copy, bps, [bX])
            cp(X[:, 0:K - 1], hal[:, j, 0:K - 1], [b_hal], [bX])
            cp(hal[:, j, 0:K - 1], X[:, TT:TT + K - 1], [bX], [b_hal])
            act(A[:], ps, AF.Identity, bps + [b_pp], [bA], scale=pp[:, l, wbase + K - 1:wbase + K],
                bias=pp[:, l, bcol:bcol + 1])
            for k in range(K - 1):
                stt(A[:], X[:, k:k + TT], pp[:, l, wbase + k:wbase + k + 1], A[:], ALU.mult, ALU.add, [bX, bA, b_pp], [bA])
            return A, bA

        def norm_transpose_out(num_ps, bnum, fac, nh, tgate, b_tg, c, extra_reads, yT, b_yT):
            on, b_on = B2[2], b_B2[2]
            w = 1024 // nh
            tt(on[:].rearrange("p (h v) -> p h v", h=nh), num_ps.rearrange("p (h v) -> p h v", h=nh),
               fac.unsqueeze(2).broadcast_to([128, nh, w]), ALU.mult, bnum + extra_reads, [b_on])
            pt, bpt = pbf()
            for j in range(8):
                trp(pt[:, j * 128:(j + 1) * 128], on[:, j * 128:(j + 1) * 128], [b_on, b_idb], bpt)
            tt(yT[:, :, c * 128:(c + 1) * 128], pt.rearrange("p (k t) -> p k t", k=8), tgate, ALU.mult,
               bpt + b_tg, [b_yT])

        def branch(l, k, yTs, b_yTs):
            gT, b_gT = A8[1], b_A8[1]

            def cons_g(j, ps, bps):
                act(gT[:, j, :], ps, AF.Sigmoid, bps + [b_pp], [b_gT], bias=pp[:, l, P_GB + k * 8 + j:P_GB + k * 8 + j + 1])
            proj_fm(win_d[l], O_GATES + k * 1024, 8, cons_g)

            def cons_z(j, ps, bps):
                if k == 0:
                    tt(merged[:, j, :], ps, gT[:, j, :], ALU.mult, bps + [b_gT], [b_merged])
                else:
                    tmp, b_tmp = acc[0], b_acc[0]
                    tt(tmp[:], ps, gT[:, j, :], ALU.mult, bps + [b_gT], [b_tmp])
                    tt(merged[:, j, :], merged[:, j, :], tmp[:], ALU.add, [b_merged, b_tmp], [b_merged])
            proj_fm(wbr_d[l, k], 0, 8, cons_z, rhsT=yTs, rb=b_yTs)

        def gla_proj(l):
            sl, bs = slab(win_d[l][:, O_GA:O_GA + 16], 8, 16)
            ps, bps = pbank()
            for kd in range(8):
                mm(ps[0:16, 0:TT], sl[:, kd, 0:16], uT[:, kd, :], kd == 0, kd == 7, [bs, b_uT], bps)
            act(gaT[0:16, :], ps[0:16, 0:TT], AF.Copy, bps, [b_gaT])
            tk.dma("sp", wab[0:17, :], wab_d[l], writes=[b_wab])
            for c in range(NCH):
                ps, bps = pbank()
                mm(ps[:, :], gaT[0:17, c * 128:(c + 1) * 128], wab[0:17, :], True, True, [b_gaT, b_wab], bps)
                act(F4[1][:, 0:512], ps, AF.Exp, bps, [b_F4[1]], scale=-1.0)
                act(la[:, c, :], F4[1][:, 0:512], AF.Ln, [b_F4[1]], [b_la], bias=1.0)
            for h in range(4):
                ps, bps = pbank()
                for c in range(NCH):
                    mm(ps[:, c * 128:(c + 1) * 128], la[:, c, h * 128:(h + 1) * 128], CST(C_TRIG), True, True,
                       [b_la, b_cst], bps)
                act(Epos[:, h, :], ps[:, 0:TT], AF.Exp, bps, [b_Epos])
                act(Eneg[:, h, :], ps[:, 0:TT], AF.Exp, bps, [b_Eneg], scale=-1.0)

            def cons_q(j, ps, bps):
                stt(qdT[:, j, :], ps, 128.0 ** -0.5, Epos[:, j, :], ALU.mult, ALU.mult, bps + [b_Epos], [b_qdT])
            proj_fm(win_d[l], O_GQ, 4, cons_q)

            def cons_k(j, ps, bps):
                tt(kiT[:, j, :], ps, Eneg[:, j, :], ALU.mult, bps + [b_Eneg], [b_kiT])
            proj_fm(win_d[l], O_GK, 4, cons_k)

            tg, b_tg = A8[0], b_A8[0]

            def cons_gg(j, ps, bps):
                act(tg[:, j, :], ps, AF.Silu, bps, [b_tg])
                ts(tg[:, j, :], tg[:, j, :], pp[:, l, P_GNW + j:P_GNW + j + 1], None, ALU.mult, None, [b_tg, b_pp], [b_tg])
            proj_fm(win_d[l], O_GG, 8, cons_gg)

            for c in range(NCH):
                ps, bps = pbank()
                mm(ps[:, :], CST(C_UG), la[:, c, :], True, True, [b_la, b_cst], bps)
                act(erev[:, c, :], ps, AF.Exp, bps, [b_erev])

            def cons_ktm(s, c, ps, bps):
                tt(kdec[:, c, :], ps, erev[:, c, :], ALU.mult, bps + [b_erev], [b_kdec])
            proj_tm(win_d[l], O_GK, 512, cons_ktm)

            def cons_v(s, c, ps, bps):
                act(vb[:, c, s * 512:(s + 1) * 512], ps, AF.Copy, bps, [b_vb])
            proj_tm(win_d[l], O_GV, 1024, cons_v)

        def gla_loop(l):
            tg, b_tg = A8[0], b_A8[0]
            tk.dma("sp", gS[l][:].rearrange("p h v -> p (h v)"), gS_d[l], reads=[b_gSd[l]], writes=[b_gS[l]])
            act(gSb[:], gS[l][:], AF.Copy, [b_gS[l]], [b_gSb])
            yield
            for c in range(NCH):
                cs = slice(c * 128, (c + 1) * 128)
                ps, bps = pbankA()
                for h in range(4):
                    mm(ps[:, h * 128:(h + 1) * 128], kiT[:, h, cs], qdT[:, h, cs], True, True, [b_kiT, b_qdT], bps)
                scT, b_scT = B2[0], b_B2[0]
                tt(scT[:, 0:512].rearrange("p (h c) -> p h c", h=4), ps.rearrange("p (h c) -> p h c", h=4),
                   CST(C_TRI).unsqueeze(1).broadcast_to([128, 4, 128]), ALU.mult, bps + [b_cst], [b_scT])
                yield
                po, bpo = pbank2()
                pof = po.rearrange("p a b -> p (a b)")
                for h in range(4):
                    mm(pof[:, h * 256:(h + 1) * 256], scT[:, h * 128:(h + 1) * 128], vb[:, c, h * 256:(h + 1) * 256], True, False,
                       [b_scT, b_vb], bpo)
                    mm(pof[:, h * 256:(h + 1) * 256], qdT[:, h, cs], gSb[:, h, :], False, True, [b_qdT, b_gSb], bpo)
                yield
                act(F4[0][:], pof, AF.Square, bpo, [b_F4[0]])
                rsum(st[:, 0:4], F4[0][:].rearrange("p (h v) -> p h v", h=4), [b_F4[0]], [b_st])
                yield
                r = rstd_from_ss(st[:, 0:4], 4, 256, [])
                yield
                norm_transpose_out(pof, bpo, r, 4, tg[:, :, cs], [b_tg], c, [b_st], A8[2], b_A8[2])
                yield
                pst, bpst = pbank2()
                pstf = pst.rearrange("p a b -> p (a b)")
                for h in range(4):
                    mm(pstf[:, h * 256:(h + 1) * 256], kdec[:, c, h * 128:(h + 1) * 128], vb[:, c, h * 256:(h + 1) * 256],
                       True, True, [b_kdec, b_vb], bpst)
                yield
                for h in range(4):
                    stt(gS[l][:, h, :], gS[l][:, h, :], Epos[:, h, c * 128 + 127:c * 128 + 128], pstf[:, h * 256:(h + 1) * 256],
                        ALU.mult, ALU.add, [b_gS[l], b_Epos] + bpst, [b_gS[l]])
                act(gSb[:], gS[l][:], AF.Copy, [b_gS[l]], [b_gSb])
                yield
            tk.dma("sp", gS_d[l], gS[l][:].rearrange("p h v -> p (h v)"), reads=[b_gS[l]], writes=[b_gSd[l]])

        def mlstm_proj(l):
            qT, b_qT, kT, b_kT = qT2, b_qT2, kT2, b_kT2

            def cons_qk(j, ps, bps):
                A, bA = conv(ps, bps, mh[l], b_mh[l], j, 4, P_MCW + j * 4, P_MCB + j, l)
                if j < 4:
                    act(qT[:, j, :], A[:], AF.Silu, [bA], [b_qT])
                else:
                    act(kT[:, j - 4, :], A[:], AF.Silu, [bA], [b_kT])
            proj_fm(win_d[l], O_MQK, 8, cons_qk)

            tg, b_tg = TG2, b_TG2

            def cons_mo(j, ps, bps):
                act(tg[:, j, :], ps, AF.Sigmoid, bps, [b_tg])
                ts(tg[:, j, :], tg[:, j, :], pp[:, l, P_MNW + j:P_MNW + j + 1], None, ALU.mult, None, [b_tg, b_pp], [b_tg])
            proj_fm(win_d[l], O_MO, 8, cons_mo)

            def cons_g(s, c, ps, bps):
                tt(sm[:, c, 0:8], ps, bp[:, l, B_BI:B_BI + 8], ALU.add, bps + [b_bp], [b_sm])
                act(sm[:, c, 12:16], sm[:, c, 4:8], AF.Exp, [b_sm], [b_sm], scale=-1.0)
                act(sm[:, c, 8:12], sm[:, c, 12:16], AF.Ln, [b_sm], [b_sm], bias=1.0)
            proj_tm(win_d[l], O_MI, 8, cons_g)

            def cons_v(s, c, ps, bps):
                act(VB2[:, c, s * 512:(s + 1) * 512], ps, AF.Copy, bps, [b_VB2])
            proj_tm(win_d[l], O_MV, 1024, cons_v)

        def mlstm_loop(l):
            qT, b_qT, kT, b_kT = qT2, b_qT2, kT2, b_kT2
            tg, b_tg = TG2, b_TG2
            vb, b_vb = VB2, b_VB2
            tk.dma("sp", mC[l][:].rearrange("p h v -> p (h v)"), mC_d[l], reads=[b_mCd[l]], writes=[b_mC[l]])
            act(mCb[:], mC[l][:], AF.Copy, [b_mC[l]], [b_mCb])
            act(mNb[:], mN[l][:], AF.Copy, [b_mN[l]], [b_mNb])
            yield
            for c in range(NCH):
                cs = slice(c * 128, (c + 1) * 128)
                ig = sm[:, c, 0:4]
                lfn = sm[:, c, 8:12]
                rhs1, b_rhs1 = F4[1], b_F4[1]
                tt(rhs1[:, 0:512].rearrange("p (h c) -> p h c", h=4), CST(C_TRI).unsqueeze(1).broadcast_to([128, 4, 128]),
                   lfn.unsqueeze(2).broadcast_to([128, 4, 128]), ALU.mult, [b_cst, b_sm], [b_rhs1])
                pL, bpL = pbankA()
                mm(pL[:, :], CST(C_UNEG), rhs1[:, 0:512], True, True, [b_cst, b_rhs1], bpL)
                yield
                Dt, b_Dt = F4[2], b_F4[2]
                for h in range(4):
                    act(Dt[:, h * 128:(h + 1) * 128], pL[:, h * 128:(h + 1) * 128], AF.Exp, bpL + [b_sm], [b_Dt],
                        bias=sm[:, c, h:h + 1])
                tt(Dt[:, 0:512].rearrange("p (h c) -> p h c", h=4), Dt[:, 0:512].rearrange("p (h c) -> p h c", h=4),
                   CST(C_MASKM).unsqueeze(1).broadcast_to([128, 4, 128]), ALU.mult, [b_Dt, b_cst], [b_Dt])
                yield
                pE, bpE = pbankA()
                mm(pE[:, :], CST(C_ONESNEG), rhs1[:, 0:512], True, True, [b_cst, b_rhs1], bpE)
                EB, b_EB = F4[3], b_F4[3]
                act(EB[:, 0:512], pE, AF.Exp, bpE, [b_EB])
                qtil, b_qtil = B2[1], b_B2[1]
                tt(qtil[:, 0:512].rearrange("p (h c) -> p h c", h=4), qT[:, :, cs], EB[:, 0:512].rearrange("p (h c) -> p h c", h=4),
                   ALU.mult, [b_qT, b_EB], [b_qtil])
                yield
                ps, bps = pbankA()
                for h in range(4):
                    mm(ps[:, h * 128:(h + 1) * 128], kT[:, h, cs], qT[:, h, cs], True, True, [b_kT, b_qT], bps)
                wT, b_wT = B2[0], b_B2[0]
                tt(wT[:, 0:512], ps, Dt[:, 0:512], ALU.mult, bps + [b_Dt], [b_wT])
                yield
                po, bpo = pbank2()
                pof = po.rearrange("p a b -> p (a b)")
                for h in range(4):
                    mm(pof[:, h * 256:(h + 1) * 256], wT[:, h * 128:(h + 1) * 128], vb[:, c, h * 256:(h + 1) * 256], True, False,
                       [b_wT, b_vb], bpo)
                    mm(pof[:, h * 256:(h + 1) * 256], qtil[:, h * 128:(h + 1) * 128], mCb[:, h, :], False, True, [b_qtil, b_mCb], bpo)
                pd, bpd = pbankA()
                for h in range(4):
                    mm(pd[:, h * 16:h * 16 + 1], wT[:, h * 128:(h + 1) * 128], onesb[:, 0:1], True, False, [b_wT, b_onesb], bpd)
                    mm(pd[:, h * 16:h * 16 + 1], qtil[:, h * 128:(h + 1) * 128], mNb[:, h:h + 1], False, True, [b_qtil, b_mNb], bpd)
                mm(pd[:, 64:68], CST(C_UNEG), lfn, True, True, [b_cst, b_sm], bpd)
                yield
                tt(st[:, 8:12], pd[:, 64:68], ig, ALU.add, bpd + [b_sm], [b_st])
                act(st[:, 8:12], st[:, 8:12], AF.Exp, [b_st], [b_st])
                yield
                vw, b_vw = B2[3], b_B2[3]
                tt(vw[:].rearrange("p (h v) -> p h v", h=4), vb[:, c, :].rearrange("p (h v) -> p h v", h=4),
                   st[:, 8:12].unsqueeze(2).broadcast_to([128, 4, 256]), ALU.mult, [b_vb, b_st], [b_vw])
                wstb, b_wstb = B2[4], b_B2[4]
                cp(wstb[:, 0:4], st[:, 8:12], [b_st], [b_wstb])
                pt, bpt = pbf()
                for h in range(4):
                    trp(pt[:, h * 128:(h + 1) * 128], kT[:, h, cs], [b_kT, b_idb], bpt)
                ktok, b_ktok = B2[5], b_B2[5]
                act(ktok[:, 0:512], pt[:, 0:512], AF.Identity, bpt, [b_ktok], scale=128.0 ** -0.5)
                yield
                den4 = pd[:, 0:64].rearrange("p (h s) -> p h s", s=16)[:, :, 0]
                ts(st[:, 20:24], den4, -1.0, 0.0, ALU.mult, ALU.add, bpd + [b_st], [b_st])
                tt(st[:, 12:16], den4, st[:, 20:24], ALU.max, bpd + [b_st], [b_st])
                ts(st[:, 12:16], st[:, 12:16], 1.0, 0.0, ALU.max, ALU.add, [b_st], [b_st])
                recip(st[:, 12:16], st[:, 12:16], [b_st], [b_st])
                act(F4[0][:], pof, AF.Square, bpo, [b_F4[0]])
                yield
                rsum(st[:, 0:4], F4[0][:].rearrange("p (h v) -> p h v", h=4), [b_F4[0]], [b_st])
                tt(st[:, 0:4], st[:, 0:4], st[:, 12:16], ALU.mult, [b_st], [b_st])
                tt(st[:, 0:4], st[:, 0:4], st[:, 12:16], ALU.mult, [b_st], [b_st])
                yield
                r = rstd_from_ss(st[:, 0:4], 4, 256, [])
                tt(st[:, 16:20], r, st[:, 12:16], ALU.mult, [b_st], [b_st])
                yield
                norm_transpose_out(pof, bpo, st[:, 16:20], 4, tg[:, :, cs], [b_tg], c, [b_st], YT2, b_YT2)
                yield
                pst, bpst = pbank2()
                pstf = pst.rearrange("p a b -> p (a b)")
                for h in range(4):
                    mm(pstf[:, h * 256:(h + 1) * 256], ktok[:, h * 128:(h + 1) * 128], vw[:, h * 256:(h + 1) * 256], True, True,
                       [b_ktok, b_vw], bpst)
                pn, bpn = pbankA()
                for h in range(4):
                    mm(pn[:, h * 16:h * 16 + 1], ktok[:, h * 128:(h + 1) * 128], wstb[:, h:h + 1], True, True, [b_ktok, b_wstb], bpn)
                yield
                EB3 = EB[:, 0:512].rearrange("p (h c) -> p h c", h=4)
                for h in range(4):
                    stt(mC[l][:, h, :], mC[l][:, h, :], EB[:, h * 128 + 127:h * 128 + 128], pstf[:, h * 256:(h + 1) * 256],
                        ALU.mult, ALU.add, [b_mC[l], b_EB] + bpst, [b_mC[l]])
                tt(mN[l][:], mN[l][:], EB3[:, :, 127], ALU.mult, [b_mN[l], b_EB], [b_mN[l]])
                tt(mN[l][:], mN[l][:], pn[:, 0:64].rearrange("p (h s) -> p h s", s=16)[:, :, 0], ALU.add, [b_mN[l]] + bpn, [b_mN[l]])
                act(mCb[:], mC[l][:], AF.Copy, [b_mC[l]], [b_mCb])
                act(mNb[:], mN[l][:], AF.Copy, [b_mN[l]], [b_mNb])
                yield
            tk.dma("sp", mC_d[l], mC[l][:].rearrange("p h v -> p (h v)"), reads=[b_mC[l]], writes=[b_mCd[l]])

        def ssd_proj(l):
            xT, b_xT = A8[0], b_A8[0]

            def cons_xbc(j, ps, bps):
                A, bA = conv(ps, bps, sh[l], b_sh[l], j, 4, P_SCW + j * 4, P_SCB + j, l)
                if j < 8:
                    act(xT[:, j, :], A[:], AF.Silu, [bA], [b_xT])
                elif j < 10:
                    act(BT[:, j - 8, :], A[:], AF.Silu, [bA], [b_BT])
                else:
                    act(CT[:, j - 10, :], A[:], AF.Silu, [bA], [b_CT])
            proj_fm(win_d[l], O_SXBC, 12, cons_xbc)

            act(aneg[:], bp[:, l, B_ALOG:B_ALOG + 16], AF.Exp, [b_bp], [b_aneg])
            ts(aneg[:], aneg[:], -1.0, None, ALU.mult, None, [b_aneg], [b_aneg])

            def cons_dt(s, c, ps, bps):
                tt(sm2[:, c, 48:64], ps, bp[:, l, B_DTB:B_DTB + 16], ALU.add, bps + [b_bp], [b_sm2])
                act(sm2[:, c, 48:64], sm2[:, c, 48:64], AF.Exp, [b_sm2], [b_sm2])
                act(sm2[:, c, 16:32], sm2[:, c, 48:64], AF.Ln, [b_sm2], [b_sm2], bias=1.0)
                tt(sm2[:, c, 32:48], sm2[:, c, 16:32], aneg[:], ALU.mult, [b_sm2, b_aneg], [b_sm2])
            proj_tm(win_d[l], O_SDT, 16, cons_dt)

            def cons_sz(s, c, ps, bps):
                act(szs[:, c, s * 512:(s + 1) * 512], ps, AF.Silu, bps, [b_szs])
            proj_tm(win_d[l], O_SZ, 1024, cons_sz)

        def ssd_loop(l):
            xT, b_xT = A8[0], b_A8[0]
            tk.dma("sp", sS[l][:], sS_d[l], reads=[b_sSd[l]], writes=[b_sS[l]])
            act(sSb[:], sS[l][:], AF.Copy, [b_sS[l]], [b_sSb])
            yield
            for c in range(NCH):
                cs = slice(c * 128, (c + 1) * 128)
                dt = sm2[:, c, 16:32]
                dA = sm2[:, c, 32:48]
                pt, bpt = pbf()
                for j in range(8):
                    trp(pt[:, j * 128:(j + 1) * 128], xT[:, j, cs], [b_xT, b_idb], bpt)
                xtok, b_xtok = B2[0], b_B2[0]
                act(xtok[:], pt, AF.Copy, bpt, [b_xtok])
                pt2, bpt2 = pbf()
                for g in range(2):
                    trp(pt2[:, g * 128:(g + 1) * 128], BT[:, g, cs], [b_BT, b_idb], bpt2)
                btok, b_btok = B2[1], b_B2[1]
                act(btok[:, 0:256], pt2[:, 0:256], AF.Copy, bpt2, [b_btok])
                yield
                pd, bpd = pbankA()
                mm(pd[:, 0:16], CST(C_TRI), dA, True, True, [b_cst, b_sm2], bpd)
                mm(pd[:, 16:32], CST(C_UGT), dA, True, True, [b_cst, b_sm2], bpd)
                mm(pd[:, 32:48], CST(C_ONES), dA, True, True, [b_cst, b_sm2], bpd)
                act(st[:, 0:48], pd[:, 0:48], AF.Exp, bpd, [b_st])
                dec_s, b_dec_s = F4[1], b_F4[1]
                cp(dec_s[:, 0:48], st[:, 0:48], [b_st], [b_dec_s])
                eb = dec_s[:, 0:16]; erv = dec_s[:, 16:32]; EBL = dec_s[:, 32:48]
                xdt, b_xdt = B2[3], b_B2[3]
                tt(xdt[:].rearrange("p (h q) -> p h q", h=16), xtok[:].rearrange("p (h q) -> p h q", h=16),
                   dt.unsqueeze(2).broadcast_to([128, 16, 64]), ALU.mult, [b_xtok, b_sm2], [b_xdt])
                xw, b_xw = B2[4], b_B2[4]
                tt(xw[:].rearrange("p (h q) -> p h q", h=16), xdt[:].rearrange("p (h q) -> p h q", h=16),
                   erv.unsqueeze(2).broadcast_to([128, 16, 64]), ALU.mult, [b_xdt, b_dec_s], [b_xw])
                yield
                ps, bps = pbankA()
                for g in range(2):
                    mm(ps[:, g * 128:(g + 1) * 128], BT[:, g, cs], CT[:, g, cs], True, True, [b_BT, b_CT], bps)
                cbm, b_cbm = cbm_t, b_cbm_t
                tt(cbm[:, 0:256].rearrange("p (g c) -> p g c", g=2), ps[:, 0:256].rearrange("p (g c) -> p g c", g=2),
                   CST(C_TRI).unsqueeze(1).broadcast_to([128, 2, 128]), ALU.mult, bps + [b_cst], [b_cbm])
                for g in range(2):
                    yield
                    rhs1, b_rhs1 = F4[2], b_F4[2]
                    tt(rhs1[:].rearrange("p (h c) -> p h c", h=8), CST(C_TRI).unsqueeze(1).broadcast_to([128, 8, 128]),
                       sm2[:, c, 32 + g * 8:32 + (g + 1) * 8].unsqueeze(2).broadcast_to([128, 8, 128]), ALU.mult,
                       [b_cst, b_sm2], [b_rhs1])
                    pL, bpL = pbank2()
                    for hh in range(2):
                        mm(pL[:, hh, :], CST(C_UGT), rhs1[:, hh * 512:(hh + 1) * 512], True, True, [b_cst, b_rhs1], bpL)
                    dec, b_dec = F4[3], b_F4[3]
                    act(dec[:], pL.rearrange("p a b -> p (a b)"), AF.Exp, bpL, [b_dec])
                    tt(mixT[:, g * 8:(g + 1) * 8, :], dec[:].rearrange("p (h c) -> p h c", h=8),
                       cbm[:, g * 128:(g + 1) * 128].unsqueeze(1).broadcast_to([128, 8, 128]), ALU.mult, [b_dec, b_cbm], [b_mixT])
                yield
                pi, bpi = pbank2()
                for g in range(2):
                    mm(pi[:, g, :], CT[:, g, cs], sSb[:, g * 512:(g + 1) * 512], True, True, [b_CT, b_sSb], bpi)
                t1, b_t1 = F4[0], b_F4[0]
                tt(t1[:].rearrange("p (h q) -> p h q", h=16), pi.rearrange("p a (h q) -> p (a h) q", q=64),
                   eb.unsqueeze(2).broadcast_to([128, 16, 64]), ALU.mult, bpi + [b_dec_s], [b_t1])
                yield
                py, bpy = pbank2()
                pyf = py.rearrange("p a b -> p (a b)")
                for h in range(16):
                    mm(pyf[:, h * 64:(h + 1) * 64], mixT[:, h, :], xdt[:, h * 64:(h + 1) * 64], True, True, [b_mixT, b_xdt], bpy)
                tt(t1[:], t1[:], pyf, ALU.add, [b_t1] + bpy, [b_t1])
                yield
                pst, bpst = pbank2()
                for g in range(2):
                    mm(pst[:, g, :], btok[:, g * 128:(g + 1) * 128], xw[:, g * 512:(g + 1) * 512], True, True, [b_btok, b_xw], bpst)
                tt(sS[l][:].rearrange("p (h q) -> p h q", h=16), sS[l][:].rearrange("p (h q) -> p h q", h=16),
                   EBL.unsqueeze(2).broadcast_to([128, 16, 64]), ALU.mult, [b_sS[l], b_dec_s], [b_sS[l]])
                tt(sS[l][:], sS[l][:], pst.rearrange("p a b -> p (a b)"), ALU.add, [b_sS[l]] + bpst, [b_sS[l]])
                act(sSb[:], sS[l][:], AF.Copy, [b_sS[l]], [b_sSb])
                yield
                t2, b_t2 = F4[2], b_F4[2]
                tt(t2[:].rearrange("p (h q) -> p h q", h=16), xtok[:].rearrange("p (h q) -> p h q", h=16),
                   bp[:, l, B_SD:B_SD + 16].unsqueeze(2).broadcast_to([128, 16, 64]), ALU.mult, [b_xtok, b_bp], [b_t2])
                tt(t1[:], t1[:], t2[:], ALU.add, [b_t1, b_t2], [b_t1])
                tt(t1[:], t1[:], szs[:, c, :], ALU.mult, [b_t1, b_szs], [b_t1])
                yield
                act(F4[3][:], t1[:], AF.Square, [b_t1], [b_F4[3]])
                rsum(st[:, 0:2], F4[3][:].rearrange("p (h v) -> p h v", h=2), [b_F4[3]], [b_st])
                yield
                r = rstd_from_ss(st[:, 0:2], 2, 512, [])
                yield
                norm_transpose_out(t1[:], [b_t1], r, 2, pp[:, l, P_SNW:P_SNW + 8].unsqueeze(2).broadcast_to([128, 8, 128]),
                                   [b_pp], c, [b_st], A8[2], b_A8[2])
                yield
            tk.dma("sp", sS_d[l], sS[l][:], reads=[b_sS[l]], writes=[b_sSd[l]])

        def outproj(l):
            mb, b_mb = A8[1], b_A8[1]
            act(mb[:], merged[:], AF.Copy, [b_merged], [b_mb])
            for half in range(2):
                sl, bs = slab(wout_d[l][:, half * 512:(half + 1) * 512], 8, 512)
                for c in range(NCH):
                    ps, bps = pbank()
                    for kd in range(8):
                        mm(ps[:, :], mb[:, kd, c * 128:(c + 1) * 128], sl[:, kd, :], kd == 0, kd == 7, [b_mb, bs], bps)
                    tt(hres[:, c, half * 512:(half + 1) * 512], hres[:, c, half * 512:(half + 1) * 512], ps, ALU.add,
                       [b_h] + bps, [b_h])

        def ffn_phase(l):
            rmsnorm_to_uT(l, P_NFFN)
            hid = A8
            j = 0
            while j < 22:
                n = min(4, 22 - j)
                slg, bsg = slab(wup_d[l][:, j * 128:(j + n) * 128], 8, n * 128)
                slv, bsv = slab(wup_d[l][:, DFF + j * 128:DFF + (j + n) * 128], 8, n * 128)
                for jj in range(n):
                    J = j + jj
                    ps, bps = pbank()
                    for kd in range(8):
                        mm(ps[:, 0:TT], slg[:, kd, jj * 128:(jj + 1) * 128], uT[:, kd, :], kd == 0, kd == 7, [bsg, b_uT], bps)
                    Ag, bAg = conv(ps[:, 0:TT], bps, fh[l], b_fh[l], J, 3, P_FCW + J * 3, P_FCB + J, l)
                    act(Ag[:], Ag[:], AF.Silu, [bAg], [bAg])
                    ps2, bps2 = pbank()
                    for kd in range(8):
                        mm(ps2[:, 0:TT], slv[:, kd, jj * 128:(jj + 1) * 128], uT[:, kd, :], kd == 0, kd == 7, [bsv, b_uT], bps2)
                    Av, bAv = conv(ps2[:, 0:TT], bps2, fh[l], b_fh[l], 22 + J, 3, P_FCW + (22 + J) * 3, P_FCB + 22 + J, l)
                    tt(hid[J // 8][:, J % 8, :], Ag[:], Av[:], ALU.mult, [bAg, bAv], [b_A8[J // 8]])
                j += n
            for half in range(2):
                pss = [pbank() for _ in range(NCH)]
                for kg in range(3):
                    nk = 8 if kg < 2 else 6
                    sl, bs = slab(wdn_d[l][kg * 1024:kg * 1024 + nk * 128, half * 512:(half + 1) * 512], nk, 512)
                    for c in range(NCH):
                        ps, bps = pss[c]
                        for kk in range(nk):
                            J = kg * 8 + kk
                            mm(ps[:, :], hid[J // 8][:, J % 8, c * 128:(c + 1) * 128], sl[:, kk, :], J == 0, J == 21,
                               [b_A8[J // 8], bs], bps)
                for c in range(NCH):
                    ps, bps = pss[c]
                    tt(hres[:, c, half * 512:(half + 1) * 512], hres[:, c, half * 512:(half + 1) * 512], ps, ALU.add,
                       [b_h] + bps, [b_h])

        def final_norm_store(t):
            nf, b_nf = F4[3], b_F4[3]
            tk.dma("sp", nf[:], nf_d, writes=[b_nf])
            for c in range(NCH):
                tk.op("dve", lambda e: e.memset(st[:, 0:1], 0.0), writes=[b_st])
                act(F4[0][:], hres[:, c, :], AF.Square, [b_h], [b_F4[0], b_st], accum=st[:, 0:1])
                r = rstd_from_ss(st[:, 0:1], 1, D, [])
                o, b_o = F4[1 + (c % 2)], b_F4[1 + (c % 2)]
                stt(o[:], hres[:, c, :], r, nf[:], ALU.mult, ALU.mult, [b_h, b_st, b_nf], [b_o])
                r0 = t * TT + c * 128
                tk.dma("sp", out_d[r0:r0 + 128, :], o[:], reads=[b_o])

        def emit():
            rr.update({"p1": 0, "a1": 0, "pb": 0, "xs": 0, "acc": 0})
            tk.dma("sp", pp[:], pp_d.rearrange("l p n -> p l n"), writes=[b_pp])
            tk.dma("sp", bp[:], bp_d.rearrange("l p n -> p l n"), writes=[b_bp])
            tk.dma("sp", cst[:], cst_d.rearrange("p (k n) -> p k n", k=NCONST), writes=[b_cst])
            tk.dma("sp", idf[:], idn_d, writes=[b_idf])
            cp(idb[:], idf[:], [b_idf], [b_idb])
            tk.op("dve", lambda e: e.memset(onesb[:], 1.0), writes=[b_onesb])
            tk.op("dve", lambda e: e.memset(gaT[:], 1.0), writes=[b_gaT])
            tk.op("dve", lambda e: e.memset(wab[:], 0.0), writes=[b_wab])
            tk.op("dve", lambda e: e.memset(F4[0][:], 0.0), writes=[b_F4[0]])
            for l in range(NL):
                for t_, b_ in ((mN[l], b_mN[l]), (mh[l], b_mh[l]), (sh[l], b_sh[l]), (fh[l], b_fh[l])):
                    tk.op("dve", lambda e: e.memset(t_[:], 0.0), writes=[b_])
                tk.dma("sp", gS_d[l], F4[0][:], reads=[b_F4[0]], writes=[b_gSd[l]])
                tk.dma("sp", mC_d[l], F4[0][:], reads=[b_F4[0]], writes=[b_mCd[l]])
                tk.dma("sp", sS_d[l], F4[0][:], reads=[b_F4[0]], writes=[b_sSd[l]])
            for t in range(NTILES):
                tk.dma("sp", hres[:], x_d[t * TT:(t + 1) * TT, :].rearrange("(c p) d -> p c d", p=128), writes=[b_h])
                for l in range(NL):
                    rmsnorm_to_uT(l, P_NMIX)
                    gla_proj(l)
                    bg_run(gla_loop(l), lambda: mlstm_proj(l))
                    bg_run(mlstm_loop(l), lambda: (branch(l, 0, A8[2], b_A8[2]), ssd_proj(l)))
                    bg_run(ssd_loop(l), lambda: branch(l, 1, YT2, b_YT2))
                    branch(l, 2, A8[2], b_A8[2])
                    outproj(l)
                    ffn_phase(l)
                final_norm_store(t)
            if not tk.dry:
                for i in range(len(tk.dsem)):
                    if tk.dcnt[i]:
                        tk._wait("sp", (i, tk.dcnt[i]))

        tk.dry = True
        emit()
        tk.dry = False
        emit()
        _bp.last_nins = tk.nins
    return nc


build_program = _bp
build_program.debug = False


def _consts():
    k = np.arange(128)[:, None]
    c = np.arange(128)[None, :]
    tri = (k <= c).astype(np.float32)
    ugt = (k > c).astype(np.float32)
    ones = np.ones((128, 128), np.float32)
    lst = [tri, ugt, tri * (-1.0 / 16.0), ugt * (-1.0 / 16.0), -ugt, ones, -ones, tri * (128.0 ** -0.5)]
    return np.ascontiguousarray(np.concatenate(lst, axis=1).astype(np.float32))


def _fm(v):
    v = np.asarray(v, np.float32)
    return v.reshape(-1, 128).T


def _pack_params(inp, NL):
    pp = np.zeros((NL, 128, NPP), np.float32)
    bp = np.zeros((NL, 128, NBP), np.float32)
    for l in range(NL):
        pp[l, :, P_NMIX:P_NMIX + 8] = _fm(inp["norm_mix"][l])
        pp[l, :, P_NFFN:P_NFFN + 8] = _fm(inp["norm_ffn"][l])
        pp[l, :, P_GNW:P_GNW + 8] = _fm(inp["gla_norm"][l])
        pp[l, :, P_MNW:P_MNW + 8] = _fm(inp["mlstm_norm"][l])
        pp[l, :, P_SNW:P_SNW + 8] = _fm(inp["ssd_norm"][l])
        w = np.asarray(inp["mlstm_conv_w"][l])
        pp[l, :, P_MCW:P_MCW + 32] = np.stack([_fm(w[k]) for k in range(4)], axis=2).reshape(128, 32)
        pp[l, :, P_MCB:P_MCB + 8] = _fm(inp["mlstm_conv_b"][l])
        w = np.asarray(inp["ssd_conv_w"][l])
        pp[l, :, P_SCW:P_SCW + 48] = np.stack([_fm(w[k]) for k in range(4)], axis=2).reshape(128, 48)
        pp[l, :, P_SCB:P_SCB + 12] = _fm(inp["ssd_conv_b"][l])
        w = np.asarray(inp["ffn_conv_w"][l])
        pp[l, :, P_FCW:P_FCW + 132] = np.stack([_fm(w[k]) for k in range(3)], axis=2).reshape(128, 132)
        pp[l, :, P_FCB:P_FCB + 44] = _fm(inp["ffn_conv_b"][l])
        pp[l, :, P_GB:P_GB + 24] = _fm(inp["gate_b"][l])
        row = np.concatenate([inp["mlstm_bi"][l], inp["mlstm_bf"][l], inp["ssd_dt_bias"][l], inp["ssd_a_log"][l],
                              inp["ssd_d"][l]]).astype(np.float32)
        bp[l] = np.broadcast_to(row[None, :], (128, NBP))
    return pp, bp


_CACHE = {}


def run(inputs, NL, B, T, NCH=2, n_cores=None):
    inp = {k: np.asarray(v) for k, v in inputs.items()}
    TT = NCH * 128
    NT = T // TT
    key = (NL, NT, NCH)
    if key not in _CACHE:
        _CACHE[key] = build_program(NL, NT, NCH)
    nc = _CACHE[key]
    pp, bp = _pack_params(inp, NL)
    wab = np.ascontiguousarray(np.concatenate([inp["gla_wa"][:NL], inp["gla_ba"][:NL, None, :]], axis=1).astype(np.float32))
    nf = np.ascontiguousarray(np.broadcast_to(inp["norm_final"].astype(np.float32)[None, :], (128, D)))
    shared = dict(
        w_in=np.ascontiguousarray(inp["w_in"][:NL], np.float32),
        w_branch=np.ascontiguousarray(inp["w_branch"][:NL], np.float32),
        w_out=np.ascontiguousarray(inp["w_out"][:NL], np.float32),
        w_up=np.ascontiguousarray(inp["w_up"][:NL], np.float32),
        w_down=np.ascontiguousarray(inp["w_down"][:NL], np.float32),
        gla_wab=wab, pp=pp, bp=bp, nf=nf, consts=_consts(), ident=np.eye(128, dtype=np.float32),
    )
    in_maps = []
    for b in range(B):
        m = dict(shared)
        m["x"] = np.ascontiguousarray(inp["x"][b, :T], np.float32)
        in_maps.append(m)
    res = run_bass_kernel_spmd(nc, in_maps, core_ids=list(range(B)))
    if _bp.debug:
        run.dbg = np.asarray(res.results[0]["dbg"])
    return np.stack([np.asarray(r["out"]) for r in res.results], axis=0).astype(np.float32)


def kernel(**inputs):
    return run(inputs, NL=4, B=4, T=4096, NCH=2)
```

```python
import numpy as np
from collections import deque
from contextlib import ExitStack
import concourse.bass as bass
import concourse.mybir as mybir
from concourse.bass_utils import run_bass_kernel_spmd

F32 = mybir.dt.float32
BF16 = mybir.dt.bfloat16
AF = mybir.ActivationFunctionType
ALU = mybir.AluOpType
AX = mybir.AxisListType

D = 1024
DIN = 11816
DFF = 2816
EPS = 1e-6
O_GQ, O_GK, O_GV, O_GA, O_GG = 0, 512, 1024, 2048, 2064
O_MQK, O_MV, O_MI, O_MO = 3088, 4112, 5136, 5144
O_SZ, O_SXBC, O_SDT, O_GATES = 6168, 7192, 8728, 8744
P_NMIX, P_NFFN, P_GNW, P_MNW, P_SNW, P_MCW, P_MCB, P_SCW, P_SCB, P_FCW, P_FCB, P_GB, NPP = \
    0, 8, 16, 24, 32, 40, 72, 80, 128, 140, 272, 316, 340
B_BI, B_DTB, B_ALOG, B_SD, NBP = 0, 8, 24, 40, 56
C_TRI, C_UGT, C_TRIG, C_UG, C_UNEG, C_ONES, C_ONESNEG, C_MASKM, NCONST = 0, 1, 2, 3, 4, 5, 6, 7, 8


class Buf:
    __slots__ = ("lw", "rd")

    def __init__(self):
        self.lw = None
        self.rd = []


class TK:
    def __init__(self, nc, es, n_dma_sems=12):
        self.nc = nc
        self.eng = {"pe": nc.tensor, "act": nc.scalar, "dve": nc.vector, "pool": nc.gpsimd, "sp": nc.sync}
        self.sem = {}
        self.cnt = {}
        for e in ["pe", "act", "dve", "pool"]:
            self.sem[e] = es.enter_context(nc.semaphore("s_" + e))
            self.cnt[e] = 0
        self.dsem = [es.enter_context(nc.semaphore("d%d" % i)) for i in range(n_dma_sems)]
        self.dcnt = [0] * n_dma_sems
        self.dnext = 0
        self.waited = {}
        self.dry = False
        self.nins = 0
        self.dry_count = 0
        self.defer_q = None
        self.pending = None
        self.pop_every = 1
        self.b_count = 0

    def _wait(self, e, dep):
        key, val = dep
        if key == e and e == "pe":
            return
        w = self.waited.setdefault(e, {})
        if w.get(key, 0) >= val:
            return
        w[key] = val
        sem = self.sem[key] if isinstance(key, str) else self.dsem[key]
        self.eng[e].wait_ge(sem, val)
        self.nins += 1

    def _deps(self, e, reads, writes):
        for b in reads:
            if b.lw is not None:
                self._wait(e, b.lw)
        for b in writes:
            if b.lw is not None:
                self._wait(e, b.lw)
            for r in b.rd:
                self._wait(e, r)

    def _mark(self, tag, reads, writes):
        for b in reads:
            b.rd.append(tag)
            if len(b.rd) > 64:
                last = {}
                for k, v in b.rd:
                    last[k] = max(last.get(k, 0), v)
                b.rd = list(last.items())
        for b in writes:
            b.lw = tag
            b.rd = []

    def _tick(self):
        if self.pending:
            self.b_count += 1
            if self.b_count >= self.pop_every:
                self.b_count = 0
                self.pending.popleft()()

    def op(self, e, fn, reads=(), writes=()):
        if self.dry:
            self.dry_count += 1
            return
        if self.defer_q is not None:
            self.defer_q.append(lambda: self._op(e, fn, reads, writes))
            return
        self._op(e, fn, reads, writes)
        self._tick()

    def dma(self, e, out, in_, reads=(), writes=()):
        if self.dry:
            self.dry_count += 1
            return
        if self.defer_q is not None:
            self.defer_q.append(lambda: self._dma(e, out, in_, reads, writes))
            return
        self._dma(e, out, in_, reads, writes)
        self._tick()

    def _op(self, e, fn, reads=(), writes=()):
        self._deps(e, reads, writes)
        ins = fn(self.eng[e])
        self.cnt[e] += 1
        self.nins += 1
        ins.then_inc(self.sem[e], 1)
        self._mark((e, self.cnt[e]), reads, writes)

    def _dma(self, e, out, in_, reads=(), writes=()):
        i = self.dnext
        self.dnext = (self.dnext + 1) % len(self.dsem)
        if self.dcnt[i] > 0:
            self._wait(e, (i, self.dcnt[i]))
        self._deps(e, reads, writes)
        ins = self.eng[e].dma_start(out=out, in_=in_)
        self.nins += 1
        self.dcnt[i] += 16
        ins.then_inc(self.dsem[i], 16)
        self._mark((i, self.dcnt[i]), reads, writes)


def build_program(NL, NTILES, NCH=2, NSLAB=3, LOOK=2):
    pass


def _bp(NL, NTILES, NCH=2, NSLAB=6, LOOK=4):
    TT = NCH * 128
    T = NTILES * TT
    nc = bass.Bass("TRN2", target_bir_lowering=False)
    dr = lambda name, shape, kind="ExternalInput": nc.dram_tensor(name, shape, F32, kind=kind).ap()
    x_d = dr("x", [T, D])
    win_d = dr("w_in", [NL, D, DIN])
    wbr_d = dr("w_branch", [NL, 3, D, D])
    wout_d = dr("w_out", [NL, D, D])
    wup_d = dr("w_up", [NL, D, 2 * DFF])
    wdn_d = dr("w_down", [NL, DFF, D])
    wab_d = dr("gla_wab", [NL, 17, 512])
    pp_d = dr("pp", [NL, 128, NPP])
    bp_d = dr("bp", [NL, 128, NBP])
    nf_d = dr("nf", [128, D])
    cst_d = dr("consts", [128, NCONST * 128])
    idn_d = dr("ident", [128, 128])
    out_d = dr("out", [T, D], kind="ExternalOutput")
    DBG = _bp.debug
    dbg_d = dr("dbg", [24, 128, 2048], kind="ExternalOutput") if DBG else None

    with ExitStack() as es:
        tk = TK(nc, es)
        _n = [0]

        def sb(shape, dt):
            _n[0] += 1
            return es.enter_context(nc.sbuf_tensor("t%d" % _n[0], shape, dt))

        hres = sb([128, NCH, D], F32); b_h = Buf()
        uT = sb([128, 8, TT], BF16); b_uT = Buf()
        xn = sb([128, D], BF16); b_xn = Buf()
        xnn = [xn, sb([128, D], BF16)]; b_xnn = [b_xn, Buf()]
        stn = [sb([128, 8], F32) for _ in range(NCH)]; b_stn = [Buf() for _ in range(NCH)]
        _gS1 = sb([128, 4, 256], F32); _bgS1 = Buf()
        _mC1 = sb([128, 4, 256], F32); _bmC1 = Buf()
        _sS1 = sb([128, 1024], F32); _bsS1 = Buf()
        gS = [_gS1] * NL; b_gS = [_bgS1] * NL
        mC = [_mC1] * NL; b_mC = [_bmC1] * NL
        gS_d = nc.dram_tensor("gS_d", [NL, 128, 1024], F32).ap(); b_gSd = [Buf() for _ in range(NL)]
        mC_d = nc.dram_tensor("mC_d", [NL, 128, 1024], F32).ap(); b_mCd = [Buf() for _ in range(NL)]
        sS_d = nc.dram_tensor("sS_d", [NL, 128, 1024], F32).ap(); b_sSd = [Buf() for _ in range(NL)]
        mN = [sb([128, 4], F32) for _ in range(NL)]; b_mN = [Buf() for _ in range(NL)]
        sS = [_sS1] * NL; b_sS = [_bsS1] * NL
        gSb = sb([128, 4, 256], BF16); b_gSb = Buf()
        mCb = sb([128, 4, 256], BF16); b_mCb = Buf()
        mNb = sb([128, 4], BF16); b_mNb = Buf()
        sSb = sb([128, 1024], BF16); b_sSb = Buf()
        mh = [sb([128, 8, 3], F32) for _ in range(NL)]; b_mh = [Buf() for _ in range(NL)]
        sh = [sb([128, 12, 3], F32) for _ in range(NL)]; b_sh = [Buf() for _ in range(NL)]
        fh = [sb([128, 44, 2], F32) for _ in range(NL)]; b_fh = [Buf() for _ in range(NL)]
        pp = sb([128, NL, NPP], F32); b_pp = Buf()
        bp = sb([128, NL, NBP], F32); b_bp = Buf()
        cst = sb([128, NCONST, 128], F32); b_cst = Buf()
        idf = sb([128, 128], F32); b_idf = Buf()
        idb = sb([128, 128], BF16); b_idb = Buf()
        onesb = sb([128, 4], BF16); b_onesb = Buf()
        slabs = [sb([128, 8, 512], BF16) for _ in range(NSLAB)]; b_slab = [Buf() for _ in range(NSLAB)]
        wab = sb([32, 512], F32); b_wab = Buf()
        gaT = sb([32, TT], F32); b_gaT = Buf()
        aneg = sb([128, 16], F32); b_aneg = Buf()
        A8 = [sb([128, 8, TT], BF16) for _ in range(3)]; b_A8 = [Buf() for _ in range(3)]
        qdT = sb([128, 4, TT], BF16); b_qdT = Buf()
        kiT = sb([128, 4, TT], BF16); b_kiT = Buf()
        qT2 = sb([128, 4, TT], BF16); b_qT2 = Buf()
        kT2 = sb([128, 4, TT], BF16); b_kT2 = Buf()
        TG2 = sb([128, 8, TT], BF16); b_TG2 = Buf()
        YT2 = sb([128, 8, TT], BF16); b_YT2 = Buf()
        VB2 = sb([128, NCH, 1024], BF16); b_VB2 = Buf()
        cbm_t = sb([128, 256], F32); b_cbm_t = Buf()
        sm2 = sb([128, NCH, 64], F32); b_sm2 = Buf()
        la = sb([128, NCH, 512], F32); b_la = Buf()
        Epos = sb([128, 4, TT], F32); b_Epos = Buf()
        Eneg = sb([128, 4, TT], F32); b_Eneg = Buf()
        merged = sb([128, 8, TT], F32); b_merged = Buf()
        vb = sb([128, NCH, 1024], BF16); b_vb = Buf()
        kdec = sb([128, NCH, 512], BF16); b_kdec = Buf()
        erev = sb([128, NCH, 512], F32); b_erev = Buf()
        BT = sb([128, 2, TT], BF16); b_BT = Buf()
        CT = sb([128, 2, TT], BF16); b_CT = Buf()
        szs = sb([128, NCH, 1024], BF16); b_szs = Buf()
        sm = sb([128, NCH, 64], F32); b_sm = Buf()
        st = sb([128, 64], F32); b_st = Buf()
        F4 = [sb([128, 1024], F32) for _ in range(4)]; b_F4 = [Buf() for _ in range(4)]
        B2 = [sb([128, 1024], BF16) for _ in range(6)]; b_B2 = [Buf() for _ in range(6)]
        mixT = sb([128, 16, 128], BF16); b_mixT = Buf()
        xs = [sb([128, TT + 4], F32) for _ in range(2)]; b_xs = [Buf() for _ in range(2)]
        acc = [sb([128, TT], F32) for _ in range(3)]; b_acc = [Buf() for _ in range(3)]
        psf = es.enter_context(nc.psum_tensor("psf", [128, 6, 512], F32)); b_psf = [Buf() for _ in range(6)]
        psb = es.enter_context(nc.psum_tensor("psb", [128, 2, 1024], BF16)); b_psb = [Buf() for _ in range(2)]
        rr = {"p1": 0, "a1": 0, "pb": 0, "xs": 0, "acc": 0}

        def pbank():
            i = rr["p1"]; rr["p1"] = (i + 1) % 2
            return psf[:, i, :], [b_psf[i]]

        def pbankA():
            i = rr["a1"]; rr["a1"] = (i + 1) % 2
            return psf[:, 2 + i, :], [b_psf[2 + i]]

        def pbank2():
            return psf[:, 4:6, :], [b_psf[4], b_psf[5]]

        bg = [None]

        def bg_step():
            g = bg[0]
            if g is not None:
                try:
                    next(g)
                except StopIteration:
                    bg[0] = None

        bg_counts = []
        bg_idx = [0]

        def bg_run(gen, fn):
            if tk.dry:
                c0 = tk.dry_count
                for _ in gen:
                    pass
                nA = tk.dry_count - c0
                c0 = tk.dry_count
                fn()
                bg_counts.append((nA, tk.dry_count - c0))
                return
            nA, nB = bg_counts[bg_idx[0]]
            bg_idx[0] += 1
            tk.defer_q = []
            for _ in gen:
                pass
            q = tk.defer_q
            tk.defer_q = None
            tk.pending = deque(q)
            tk.pop_every = 1
            tk.b_count = 0
            fn()
            while tk.pending:
                tk.pending.popleft()()
            tk.pending = None

        def pbf():
            i = rr["pb"]; rr["pb"] = (i + 1) % 2
            return psb[:, i, :], [b_psb[i]]

        def CST(i):
            return cst[:, i, :]

        dbgbuf = sb([128, 2048], F32) if DBG else None
        b_dbg = Buf()

        def dump(idx, ap, rbufs, n):
            if not DBG:
                return
            cp(dbgbuf[:, 0:n], ap, rbufs, [b_dbg])
            tk.dma("sp", dbg_d[idx, :, 0:n], dbgbuf[:, 0:n], reads=[b_dbg])

        slab_specs = []
        slab_state = {"next": 0, "issued": 0}

        def issue_slab(i):
            src, nk, n = slab_specs[i]
            bi = i % NSLAB
            tk.dma("pool", slabs[bi][:, 0:nk, 0:n], src.rearrange("(k p) n -> p k n", p=128), writes=[b_slab[bi]])

        def slab(src, nk, n):
            if tk.dry:
                slab_specs.append((src, nk, n))
                return slabs[0], b_slab[0]
            i = slab_state["next"]; slab_state["next"] += 1
            lim = min(i + LOOK, len(slab_specs) - 1)
            while slab_state["issued"] <= lim:
                issue_slab(slab_state["issued"]); slab_state["issued"] += 1
            return slabs[i % NSLAB], b_slab[i % NSLAB]

        def mm(out, lhsT, rhs, start, stop, reads, writes):
            tk.op("pe", lambda e: e.matmul(out, lhsT=lhsT, rhs=rhs, start=start, stop=stop), reads=reads, writes=writes)

        def act(out, in_, func, reads, writes, scale=1.0, bias=None, accum=None):
            kw = {}
            if bias is not None:
                kw["bias"] = bias
            if accum is not None:
                kw["accum_out"] = accum
            tk.op("act", lambda e: e.activation(out=out, in_=in_, func=func, scale=scale, **kw), reads=reads, writes=writes)

        def tt(out, in0, in1, op, reads, writes):
            tk.op("dve", lambda e: e.tensor_tensor(out=out, in0=in0, in1=in1, op=op), reads=reads, writes=writes)

        def stt(out, in0, scalar, in1, op0, op1, reads, writes):
            tk.op("dve", lambda e: e.scalar_tensor_tensor(out=out, in0=in0, scalar=scalar, in1=in1, op0=op0, op1=op1),
                  reads=reads, writes=writes)

        def ts(out, in0, s1, s2, op0, op1, reads, writes):
            if s2 is None:
                tk.op("dve", lambda e: e.tensor_scalar(out=out, in0=in0, scalar1=s1, scalar2=0.0, op0=op0, op1=ALU.add), reads=reads, writes=writes)
            else:
                tk.op("dve", lambda e: e.tensor_scalar(out=out, in0=in0, scalar1=s1, scalar2=s2, op0=op0, op1=op1),
                      reads=reads, writes=writes)

        def trp(out, in_, reads, writes):
            tk.op("pe", lambda e: e.transpose(out=out, in_=in_, identity=idb[:]), reads=reads, writes=writes)

        def rsum(out, in_, reads, writes):
            tk.op("dve", lambda e: e.reduce_sum(out=out, in_=in_, axis=AX.X), reads=reads, writes=writes)

        def recip(out, in_, reads, writes):
            tk.op("dve", lambda e: e.reciprocal(out=out, in_=in_), reads=reads, writes=writes)

        def cp(out, in_, reads, writes):
            tk.op("dve", lambda e: e.tensor_copy(out=out, in_=in_), reads=reads, writes=writes)

        def proj_fm(wsrc, col0, nchunks, consume, rhsT=None, rb=None):
            rhsT = uT if rhsT is None else rhsT
            rb = b_uT if rb is None else rb
            j = 0
            while j < nchunks:
                n = min(4, nchunks - j)
                sl, bs = slab(wsrc[:, col0 + j * 128: col0 + (j + n) * 128], 8, n * 128)
                for jj in range(n):
                    ps, bps = pbank()
                    for kd in range(8):
                        mm(ps[:, 0:TT], sl[:, kd, jj * 128:(jj + 1) * 128], rhsT[:, kd, :], kd == 0, kd == 7,
                           [bs, rb], bps)
                    consume(j + jj, ps[:, 0:TT], bps)
                    bg_step()
                j += n

        def proj_tm(wsrc, col0, ncols, consume):
            s = 0
            while s * 512 < ncols:
                n = min(512, ncols - s * 512)
                sl, bs = slab(wsrc[:, col0 + s * 512: col0 + s * 512 + n], 8, n)
                for c in range(NCH):
                    ps, bps = pbank()
                    for kd in range(8):
                        mm(ps[:, 0:n], uT[:, kd, c * 128:(c + 1) * 128], sl[:, kd, 0:n], kd == 0, kd == 7, [bs, b_uT], bps)
                    consume(s, c, ps[:, 0:n], bps)
                    bg_step()
                s += 1

        def rstd_from_ss(ss_ap, n, width, reads):
            o = st[:, 32:32 + n]
            ts(o, ss_ap, 1.0 / width, EPS, ALU.mult, ALU.add, reads + [b_st], [b_st])
            act(o, o, AF.Ln, [b_st], [b_st])
            act(o, o, AF.Exp, [b_st], [b_st], scale=-0.5)
            return o

        def rmsnorm_to_uT(l, pcol):
            pts = []
            for c in range(NCH):
                tk.op("dve", lambda e: e.memset(stn[c][:, 0:1], 0.0), writes=[b_stn[c]])
            for c in range(NCH):
                act(F4[c % 4][:], hres[:, c, :], AF.Square, [b_h], [b_F4[c % 4], b_stn[c]], accum=stn[c][:, 0:1])
            for c in range(NCH):
                ts(stn[c][:, 1:2], stn[c][:, 0:1], 1.0 / D, EPS, ALU.mult, ALU.add, [b_stn[c]], [b_stn[c]])
            for c in range(NCH):
                act(stn[c][:, 1:2], stn[c][:, 1:2], AF.Ln, [b_stn[c]], [b_stn[c]])
            for c in range(NCH):
                act(stn[c][:, 1:2], stn[c][:, 1:2], AF.Exp, [b_stn[c]], [b_stn[c]], scale=-0.5)
            for c in range(NCH):
                act(xnn[c % 2][:], hres[:, c, :], AF.Identity, [b_h, b_stn[c]], [b_xnn[c % 2]], scale=stn[c][:, 1:2])
                pt, bpt = pbf()
                pts.append((pt, bpt))
                for kd in range(8):
                    trp(pt[:, kd * 128:(kd + 1) * 128], xnn[c % 2][:, kd * 128:(kd + 1) * 128], [b_xnn[c % 2], b_idb], bpt)
            for c in range(NCH):
                pt, bpt = pts[c]
                tt(uT[:, :, c * 128:(c + 1) * 128], pt.rearrange("p (k t) -> p k t", k=8),
                   pp[:, l, pcol:pcol + 8].unsqueeze(2).broadcast_to([128, 8, 128]), ALU.mult, bpt + [b_pp], [b_uT])

        def conv(ps, bps, hal, b_hal, j, K, wbase, bcol, l):
            i = rr["xs"]; rr["xs"] = (i + 1) % 2
            a = rr["acc"]; rr["acc"] = (a + 1) % 3
            X, bX = xs[i], b_xs[i]
            A, bA = acc[a], b_acc[a]
            act(X[:, K - 1:K - 1 + TT], ps, AF.Copy, bps, [bX])
            cp(X[:, 0:K - 1], hal[:, j, 0:K - 1], [b_hal], [bX])
            cp(hal[:, j, 0:K - 1], X[:, TT:TT + K - 1], [bX], [b_hal])
            act(A[:], ps, AF.Identity, bps + [b_pp], [bA], scale=pp[:, l, wbase + K - 1:wbase + K],
                bias=pp[:, l, bcol:bcol + 1])
            for k in range(K - 1):
                stt(A[:], X[:, k:k + TT], pp[:, l, wbase + k:wbase + k + 1], A[:], ALU.mult, ALU.add, [bX, bA, b_pp], [bA])
            return A, bA

        def norm_transpose_out(num_ps, bnum, fac, nh, tgate, b_tg, c, extra_reads, yT, b_yT):
            on, b_on = B2[2], b_B2[2]
            w = 1024 // nh
            tt(on[:].rearrange("p (h v) -> p h v", h=nh), num_ps.rearrange("p (h v) -> p h v", h=nh),
               fac.unsqueeze(2).broadcast_to([128, nh, w]), ALU.mult, bnum + extra_reads, [b_on])
            pt, bpt = pbf()
            for j in range(8):
                trp(pt[:, j * 128:(j + 1) * 128], on[:, j * 128:(j + 1) * 128], [b_on, b_idb], bpt)
            tt(yT[:, :, c * 128:(c + 1) * 128], pt.rearrange("p (k t) -> p k t", k=8), tgate, ALU.mult,
               bpt + b_tg, [b_yT])

        def branch(l, k, yTs, b_yTs):
            gT, b_gT = A8[1], b_A8[1]

            def cons_g(j, ps, bps):
                act(gT[:, j, :], ps, AF.Sigmoid, bps + [b_pp], [b_gT], bias=pp[:, l, P_GB + k * 8 + j:P_GB + k * 8 + j + 1])
            proj_fm(win_d[l], O_GATES + k * 1024, 8, cons_g)

            def cons_z(j, ps, bps):
                if k == 0:
                    tt(merged[:, j, :], ps, gT[:, j, :], ALU.mult, bps + [b_gT], [b_merged])
                else:
                    tmp, b_tmp = acc[0], b_acc[0]
                    tt(tmp[:], ps, gT[:, j, :], ALU.mult, bps + [b_gT], [b_tmp])
                    tt(merged[:, j, :], merged[:, j, :], tmp[:], ALU.add, [b_merged, b_tmp], [b_merged])
            proj_fm(wbr_d[l, k], 0, 8, cons_z, rhsT=yTs, rb=b_yTs)

        def gla_proj(l):
            sl, bs = slab(win_d[l][:, O_GA:O_GA + 16], 8, 16)
            ps, bps = pbank()
            for kd in range(8):
                mm(ps[0:16, 0:TT], sl[:, kd, 0:16], uT[:, kd, :], kd == 0, kd == 7, [bs, b_uT], bps)
            act(gaT[0:16, :], ps[0:16, 0:TT], AF.Copy, bps, [b_gaT])
            tk.dma("sp", wab[0:17, :], wab_d[l], writes=[b_wab])
            for c in range(NCH):
                ps, bps = pbank()
                mm(ps[:, :], gaT[0:17, c * 128:(c + 1) * 128], wab[0:17, :], True, True, [b_gaT, b_wab], bps)
                act(F4[1][:, 0:512], ps, AF.Exp, bps, [b_F4[1]], scale=-1.0)
                act(la[:, c, :], F4[1][:, 0:512], AF.Ln, [b_F4[1]], [b_la], bias=1.0)
            for h in range(4):
                ps, bps = pbank()
                for c in range(NCH):
                    mm(ps[:, c * 128:(c + 1) * 128], la[:, c, h * 128:(h + 1) * 128], CST(C_TRIG), True, True,
                       [b_la, b_cst], bps)
                act(Epos[:, h, :], ps[:, 0:TT], AF.Exp, bps, [b_Epos])
                act(Eneg[:, h, :], ps[:, 0:TT], AF.Exp, bps, [b_Eneg], scale=-1.0)

            def cons_q(j, ps, bps):
                stt(qdT[:, j, :], ps, 128.0 ** -0.5, Epos[:, j, :], ALU.mult, ALU.mult, bps + [b_Epos], [b_qdT])
            proj_fm(win_d[l], O_GQ, 4, cons_q)

            def cons_k(j, ps, bps):
                tt(kiT[:, j, :], ps, Eneg[:, j, :], ALU.mult, bps + [b_Eneg], [b_kiT])
            proj_fm(win_d[l], O_GK, 4, cons_k)

            tg, b_tg = A8[0], b_A8[0]

            def cons_gg(j, ps, bps):
                act(tg[:, j, :], ps, AF.Silu, bps, [b_tg])
                ts(tg[:, j, :], tg[:, j, :], pp[:, l, P_GNW + j:P_GNW + j + 1], None, ALU.mult, None, [b_tg, b_pp], [b_tg])
            proj_fm(win_d[l], O_GG, 8, cons_gg)

            for c in range(NCH):
                ps, bps = pbank()
                mm(ps[:, :], CST(C_UG), la[:, c, :], True, True, [b_la, b_cst], bps)
                act(erev[:, c, :], ps, AF.Exp, bps, [b_erev])

            def cons_ktm(s, c, ps, bps):
                tt(kdec[:, c, :], ps, erev[:, c, :], ALU.mult, bps + [b_erev], [b_kdec])
            proj_tm(win_d[l], O_GK, 512, cons_ktm)

            def cons_v(s, c, ps, bps):
                act(vb[:, c, s * 512:(s + 1) * 512], ps, AF.Copy, bps, [b_vb])
            proj_tm(win_d[l], O_GV, 1024, cons_v)

        def gla_loop(l):
            tg, b_tg = A8[0], b_A8[0]
            tk.dma("sp", gS[l][:].rearrange("p h v -> p (h v)"), gS_d[l], reads=[b_gSd[l]], writes=[b_gS[l]])
            act(gSb[:], gS[l][:], AF.Copy, [b_gS[l]], [b_gSb])
            yield
            for c in range(NCH):
                cs = slice(c * 128, (c + 1) * 128)
                ps, bps = pbankA()
                for h in range(4):
                    mm(ps[:, h * 128:(h + 1) * 128], kiT[:, h, cs], qdT[:, h, cs], True, True, [b_kiT, b_qdT], bps)
                scT, b_scT = B2[0], b_B2[0]
                tt(scT[:, 0:512].rearrange("p (h c) -> p h c", h=4), ps.rearrange("p (h c) -> p h c", h=4),
                   CST(C_TRI).unsqueeze(1).broadcast_to([128, 4, 128]), ALU.mult, bps + [b_cst], [b_scT])
                yield
                po, bpo = pbank2()
                pof = po.rearrange("p a b -> p (a b)")
                for h in range(4):
                    mm(pof[:, h * 256:(h + 1) * 256], scT[:, h * 128:(h + 1) * 128], vb[:, c, h * 256:(h + 1) * 256], True, False,
                       [b_scT, b_vb], bpo)
                    mm(pof[:, h * 256:(h + 1) * 256], qdT[:, h, cs], gSb[:, h, :], False, True, [b_qdT, b_gSb], bpo)
                yield
                act(F4[0][:], pof, AF.Square, bpo, [b_F4[0]])
                rsum(st[:, 0:4], F4[0][:].rearrange("p (h v) -> p h v", h=4), [b_F4[0]], [b_st])
                yield
                r = rstd_from_ss(st[:, 0:4], 4, 256, [])
                yield
                norm_transpose_out(pof, bpo, r, 4, tg[:, :, cs], [b_tg], c, [b_st], A8[2], b_A8[2])
                yield
                pst, bpst = pbank2()
                pstf = pst.rearrange("p a b -> p (a b)")
                for h in range(4):
                    mm(pstf[:, h * 256:(h + 1) * 256], kdec[:, c, h * 128:(h + 1) * 128], vb[:, c, h * 256:(h + 1) * 256],
                       True, True, [b_kdec, b_vb], bpst)
                yield
                for h in range(4):
                    stt(gS[l][:, h, :], gS[l][:, h, :], Epos[:, h, c * 128 + 127:c * 128 + 128], pstf[:, h * 256:(h + 1) * 256],
                        ALU.mult, ALU.add, [b_gS[l], b_Epos] + bpst, [b_gS[l]])
                act(gSb[:], gS[l][:], AF.Copy, [b_gS[l]], [b_gSb])
                yield
            tk.dma("sp", gS_d[l], gS[l][:].rearrange("p h v -> p (h v)"), reads=[b_gS[l]], writes=[b_gSd[l]])

        def mlstm_proj(l):
            qT, b_qT, kT, b_kT = qT2, b_qT2, kT2, b_kT2

            def cons_qk(j, ps, bps):
                A, bA = conv(ps, bps, mh[l], b_mh[l], j, 4, P_MCW + j * 4, P_MCB + j, l)
                if j < 4:
                    act(qT[:, j, :], A[:], AF.Silu, [bA], [b_qT])
                else:
                    act(kT[:, j - 4, :], A[:], AF.Silu, [bA], [b_kT])
            proj_fm(win_d[l], O_MQK, 8, cons_qk)

            tg, b_tg = TG2, b_TG2

            def cons_mo(j, ps, bps):
                act(tg[:, j, :], ps, AF.Sigmoid, bps, [b_tg])
                ts(tg[:, j, :], tg[:, j, :], pp[:, l, P_MNW + j:P_MNW + j + 1], None, ALU.mult, None, [b_tg, b_pp], [b_tg])
            proj_fm(win_d[l], O_MO, 8, cons_mo)

            def cons_g(s, c, ps, bps):
                tt(sm[:, c, 0:8], ps, bp[:, l, B_BI:B_BI + 8], ALU.add, bps + [b_bp], [b_sm])
                act(sm[:, c, 12:16], sm[:, c, 4:8], AF.Exp, [b_sm], [b_sm], scale=-1.0)
                act(sm[:, c, 8:12], sm[:, c, 12:16], AF.Ln, [b_sm], [b_sm], bias=1.0)
            proj_tm(win_d[l], O_MI, 8, cons_g)

            def cons_v(s, c, ps, bps):
                act(VB2[:, c, s * 512:(s + 1) * 512], ps, AF.Copy, bps, [b_VB2])
            proj_tm(win_d[l], O_MV, 1024, cons_v)

        def mlstm_loop(l):
            qT, b_qT, kT, b_kT = qT2, b_qT2, kT2, b_kT2
            tg, b_tg = TG2, b_TG2
            vb, b_vb = VB2, b_VB2
            tk.dma("sp", mC[l][:].rearrange("p h v -> p (h v)"), mC_d[l], reads=[b_mCd[l]], writes=[b_mC[l]])
            act(mCb[:], mC[l][:], AF.Copy, [b_mC[l]], [b_mCb])
            act(mNb[:], mN[l][:], AF.Copy, [b_mN[l]], [b_mNb])
            yield
            for c in range(NCH):
                cs = slice(c * 128, (c + 1) * 128)
                ig = sm[:, c, 0:4]
                lfn = sm[:, c, 8:12]
                rhs1, b_rhs1 = F4[1], b_F4[1]
                tt(rhs1[:, 0:512].rearrange("p (h c) -> p h c", h=4), CST(C_TRI).unsqueeze(1).broadcast_to([128, 4, 128]),
                   lfn.unsqueeze(2).broadcast_to([128, 4, 128]), ALU.mult, [b_cst, b_sm], [b_rhs1])
                pL, bpL = pbankA()
                mm(pL[:, :], CST(C_UNEG), rhs1[:, 0:512], True, True, [b_cst, b_rhs1], bpL)
                yield
                Dt, b_Dt = F4[2], b_F4[2]
                for h in range(4):
                    act(Dt[:, h * 128:(h + 1) * 128], pL[:, h * 128:(h + 1) * 128], AF.Exp, bpL + [b_sm], [b_Dt],
                        bias=sm[:, c, h:h + 1])
                tt(Dt[:, 0:512].rearrange("p (h c) -> p h c", h=4), Dt[:, 0:512].rearrange("p (h c) -> p h c", h=4),
                   CST(C_MASKM).unsqueeze(1).broadcast_to([128, 4, 128]), ALU.mult, [b_Dt, b_cst], [b_Dt])
                yield
                pE, bpE = pbankA()
                mm(pE[:, :], CST(C_ONESNEG), rhs1[:, 0:512], True, True, [b_cst, b_rhs1], bpE)
                EB, b_EB = F4[3], b_F4[3]
                act(EB[:, 0:512], pE, AF.Exp, bpE, [b_EB])
                qtil, b_qtil = B2[1], b_B2[1]
                tt(qtil[:, 0:512].rearrange("p (h c) -> p h c", h=4), qT[:, :, cs], EB[:, 0:512].rearrange("p (h c) -> p h c", h=4),
                   ALU.mult, [b_qT, b_EB], [b_qtil])
                yield
                ps, bps = pbankA()
                for h in range(4):
                    mm(ps[:, h * 128:(h + 1) * 128], kT[:, h, cs], qT[:, h, cs], True, True, [b_kT, b_qT], bps)
                wT, b_wT = B2[0], b_B2[0]
                tt(wT[:, 0:512], ps, Dt[:, 0:512], ALU.mult, bps + [b_Dt], [b_wT])
                yield
                po, bpo = pbank2()
                pof = po.rearrange("p a b -> p (a b)")
                for h in range(4):
                    mm(pof[:, h * 256:(h + 1) * 256], wT[:, h * 128:(h + 1) * 128], vb[:, c, h * 256:(h + 1) * 256], True, False,
                       [b_wT, b_vb], bpo)
                    mm(pof[:, h * 256:(h + 1) * 256], qtil[:, h * 128:(h + 1) * 128], mCb[:, h, :], False, True, [b_qtil, b_mCb], bpo)
                pd, bpd = pbankA()
                for h in range(4):
                    mm(pd[:, h * 16:h * 16 + 1], wT[:, h * 128:(h + 1) * 128], onesb[:, 0:1], True, False, [b_wT, b_onesb], bpd)
                    mm(pd[:, h * 16:h * 16 + 1], qtil[:, h * 128:(h + 1) * 128], mNb[:, h:h + 1], False, True, [b_qtil, b_mNb], bpd)
                mm(pd[:, 64:68], CST(C_UNEG), lfn, True, True, [b_cst, b_sm], bpd)
                yield
                tt(st[:, 8:12], pd[:, 64:68], ig, ALU.add, bpd + [b_sm], [b_st])
                act(st[:, 8:12], st[:, 8:12], AF.Exp, [b_st], [b_st])
                yield
                vw, b_vw = B2[3], b_B2[3]
                tt(vw[:].rearrange("p (h v) -> p h v", h=4), vb[:, c, :].rearrange("p (h v) -> p h v", h=4),
                   st[:, 8:12].unsqueeze(2).broadcast_to([128, 4, 256]), ALU.mult, [b_vb, b_st], [b_vw])
                wstb, b_wstb = B2[4], b_B2[4]
                cp(wstb[:, 0:4], st[:, 8:12], [b_st], [b_wstb])
                pt, bpt = pbf()
                for h in range(4):
                    trp(pt[:, h * 128:(h + 1) * 128], kT[:, h, cs], [b_kT, b_idb], bpt)
                ktok, b_ktok = B2[5], b_B2[5]
                act(ktok[:, 0:512], pt[:, 0:512], AF.Identity, bpt, [b_ktok], scale=128.0 ** -0.5)
                yield
                den4 = pd[:, 0:64].rearrange("p (h s) -> p h s", s=16)[:, :, 0]
                ts(st[:, 20:24], den4, -1.0, 0.0, ALU.mult, ALU.add, bpd + [b_st], [b_st])
                tt(st[:, 12:16], den4, st[:, 20:24], ALU.max, bpd + [b_st], [b_st])
                ts(st[:, 12:16], st[:, 12:16], 1.0, 0.0, ALU.max, ALU.add, [b_st], [b_st])
                recip(st[:, 12:16], st[:, 12:16], [b_st], [b_st])
                act(F4[0][:], pof, AF.Square, bpo, [b_F4[0]])
                yield
                rsum(st[:, 0:4], F4[0][:].rearrange("p (h v) -> p h v", h=4), [b_F4[0]], [b_st])
                tt(st[:, 0:4], st[:, 0:4], st[:, 12:16], ALU.mult, [b_st], [b_st])
                tt(st[:, 0:4], st[:, 0:4], st[:, 12:16], ALU.mult, [b_st], [b_st])
                yield
                r = rstd_from_ss(st[:, 0:4], 4, 256, [])
                tt(st[:, 16:20], r, st[:, 12:16], ALU.mult, [b_st], [b_st])
                yield
                norm_transpose_out(pof, bpo, st[:, 16:20], 4, tg[:, :, cs], [b_tg], c, [b_st], YT2, b_YT2)
                yield
                pst, bpst = pbank2()
                pstf = pst.rearrange("p a b -> p (a b)")
                for h in range(4):
                    mm(pstf[:, h * 256:(h + 1) * 256], ktok[:, h * 128:(h + 1) * 128], vw[:, h * 256:(h + 1) * 256], True, True,
                       [b_ktok, b_vw], bpst)
                pn, bpn = pbankA()
                for h in range(4):
                    mm(pn[:, h * 16:h * 16 + 1], ktok[:, h * 128:(h + 1) * 128], wstb[:, h:h + 1], True, True, [b_ktok, b_wstb], bpn)
                yield
                EB3 = EB[:, 0:512].rearrange("p (h c) -> p h c", h=4)
                for h in range(4):
                    stt(mC[l][:, h, :], mC[l][:, h, :], EB[:, h * 128 + 127:h * 128 + 128], pstf[:, h * 256:(h + 1) * 256],
                        ALU.mult, ALU.add, [b_mC[l], b_EB] + bpst, [b_mC[l]])
                tt(mN[l][:], mN[l][:], EB3[:, :, 127], ALU.mult, [b_mN[l], b_EB], [b_mN[l]])
                tt(mN[l][:], mN[l][:], pn[:, 0:64].rearrange("p (h s) -> p h s", s=16)[:, :, 0], ALU.add, [b_mN[l]] + bpn, [b_mN[l]])
                act(mCb[:], mC[l][:], AF.Copy, [b_mC[l]], [b_mCb])
                act(mNb[:], mN[l][:], AF.Copy, [b_mN[l]], [b_mNb])
                yield
            tk.dma("sp", mC_d[l], mC[l][:].rearrange("p h v -> p (h v)"), reads=[b_mC[l]], writes=[b_mCd[l]])

        def ssd_proj(l):
            xT, b_xT = A8[0], b_A8[0]

            def cons_xbc(j, ps, bps):
                A, bA = conv(ps, bps, sh[l], b_sh[l], j, 4, P_SCW + j * 4, P_SCB + j, l)
                if j < 8:
                    act(xT[:, j, :], A[:], AF.Silu, [bA], [b_xT])
                elif j < 10:
                    act(BT[:, j - 8, :], A[:], AF.Silu, [bA], [b_BT])
                else:
                    act(CT[:, j - 10, :], A[:], AF.Silu, [bA], [b_CT])
            proj_fm(win_d[l], O_SXBC, 12, cons_xbc)

            act(aneg[:], bp[:, l, B_ALOG:B_ALOG + 16], AF.Exp, [b_bp], [b_aneg])
            ts(aneg[:], aneg[:], -1.0, None, ALU.mult, None, [b_aneg], [b_aneg])

            def cons_dt(s, c, ps, bps):
                tt(sm2[:, c, 48:64], ps, bp[:, l, B_DTB:B_DTB + 16], ALU.add, bps + [b_bp], [b_sm2])
                act(sm2[:, c, 48:64], sm2[:, c, 48:64], AF.Exp, [b_sm2], [b_sm2])
                act(sm2[:, c, 16:32], sm2[:, c, 48:64], AF.Ln, [b_sm2], [b_sm2], bias=1.0)
                tt(sm2[:, c, 32:48], sm2[:, c, 16:32], aneg[:], ALU.mult, [b_sm2, b_aneg], [b_sm2])
            proj_tm(win_d[l], O_SDT, 16, cons_dt)

            def cons_sz(s, c, ps, bps):
                act(szs[:, c, s * 512:(s + 1) * 512], ps, AF.Silu, bps, [b_szs])
            proj_tm(win_d[l], O_SZ, 1024, cons_sz)

        def ssd_loop(l):
            xT, b_xT = A8[0], b_A8[0]
            tk.dma("sp", sS[l][:], sS_d[l], reads=[b_sSd[l]], writes=[b_sS[l]])
            act(sSb[:], sS[l][:], AF.Copy, [b_sS[l]], [b_sSb])
            yield
            for c in range(NCH):
                cs = slice(c * 128, (c + 1) * 128)
                dt = sm2[:, c, 16:32]
                dA = sm2[:, c, 32:48]
                pt, bpt = pbf()
                for j in range(8):
                    trp(pt[:, j * 128:(j + 1) * 128], xT[:, j, cs], [b_xT, b_idb], bpt)
                xtok, b_xtok = B2[0], b_B2[0]
                act(xtok[:], pt, AF.Copy, bpt, [b_xtok])
                pt2, bpt2 = pbf()
                for g in range(2):
                    trp(pt2[:, g * 128:(g + 1) * 128], BT[:, g, cs], [b_BT, b_idb], bpt2)
                btok, b_btok = B2[1], b_B2[1]
                act(btok[:, 0:256], pt2[:, 0:256], AF.Copy, bpt2, [b_btok])
                yield
                pd, bpd = pbankA()
                mm(pd[:, 0:16], CST(C_TRI), dA, True, True, [b_cst, b_sm2], bpd)
                mm(pd[:, 16:32], CST(C_UGT), dA, True, True, [b_cst, b_sm2], bpd)
                mm(pd[:, 32:48], CST(C_ONES), dA, True, True, [b_cst, b_sm2], bpd)
                act(st[:, 0:48], pd[:, 0:48], AF.Exp, bpd, [b_st])
                dec_s, b_dec_s = F4[1], b_F4[1]
                cp(dec_s[:, 0:48], st[:, 0:48], [b_st], [b_dec_s])
                eb = dec_s[:, 0:16]; erv = dec_s[:, 16:32]; EBL = dec_s[:, 32:48]
                xdt, b_xdt = B2[3], b_B2[3]
                tt(xdt[:].rearrange("p (h q) -> p h q", h=16), xtok[:].rearrange("p (h q) -> p h q", h=16),
                   dt.unsqueeze(2).broadcast_to([128, 16, 64]), ALU.mult, [b_xtok, b_sm2], [b_xdt])
                xw, b_xw = B2[4], b_B2[4]
                tt(xw[:].rearrange("p (h q) -> p h q", h=16), xdt[:].rearrange("p (h q) -> p h q", h=16),
                   erv.unsqueeze(2).broadcast_to([128, 16, 64]), ALU.mult, [b_xdt, b_dec_s], [b_xw])
                yield
                ps, bps = pbankA()
                for g in range(2):
                    mm(ps[:, g * 128:(g + 1) * 128], BT[:, g, cs], CT[:, g, cs], True, True, [b_BT, b_CT], bps)
                cbm, b_cbm = cbm_t, b_cbm_t
                tt(cbm[:, 0:256].rearrange("p (g c) -> p g c", g=2), ps[:, 0:256].rearrange("p (g c) -> p g c", g=2),
                   CST(C_TRI).unsqueeze(1).broadcast_to([128, 2, 128]), ALU.mult, bps + [b_cst], [b_cbm])
                for g in range(2):
                    yield
                    rhs1, b_rhs1 = F4[2], b_F4[2]
                    tt(rhs1[:].rearrange("p (h c) -> p h c", h=8), CST(C_TRI).unsqueeze(1).broadcast_to([128, 8, 128]),
                       sm2[:, c, 32 + g * 8:32 + (g + 1) * 8].unsqueeze(2).broadcast_to([128, 8, 128]), ALU.mult,
                       [b_cst, b_sm2], [b_rhs1])
                    pL, bpL = pbank2()
                    for hh in range(2):
                        mm(pL[:, hh, :], CST(C_UGT), rhs1[:, hh * 512:(hh + 1) * 512], True, True, [b_cst, b_rhs1], bpL)
                    dec, b_dec = F4[3], b_F4[3]
                    act(dec[:], pL.rearrange("p a b -> p (a b)"), AF.Exp, bpL, [b_dec])
                    tt(mixT[:, g * 8:(g + 1) * 8, :], dec[:].rearrange("p (h c) -> p h c", h=8),
                       cbm[:, g * 128:(g + 1) * 128].unsqueeze(1).broadcast_to([128, 8, 128]), ALU.mult, [b_dec, b_cbm], [b_mixT])
                yield
                pi, bpi = pbank2()
                for g in range(2):
                    mm(pi[:, g, :], CT[:, g, cs], sSb[:, g * 512:(g + 1) * 512], True, True, [b_CT, b_sSb], bpi)
                t1, b_t1 = F4[0], b_F4[0]
                tt(t1[:].rearrange("p (h q) -> p h q", h=16), pi.rearrange("p a (h q) -> p (a h) q", q=64),
                   eb.unsqueeze(2).broadcast_to([128, 16, 64]), ALU.mult, bpi + [b_dec_s], [b_t1])
                yield
                py, bpy = pbank2()
                pyf = py.rearrange("p a b -> p (a b)")
                for h in range(16):
                    mm(pyf[:, h * 64:(h + 1) * 64], mixT[:, h, :], xdt[:, h * 64:(h + 1) * 64], True, True, [b_mixT, b_xdt], bpy)
                tt(t1[:], t1[:], pyf, ALU.add, [b_t1] + bpy, [b_t1])
                yield
                pst, bpst = pbank2()
                for g in range(2):
                    mm(pst[:, g, :], btok[:, g * 128:(g + 1) * 128], xw[:, g * 512:(g + 1) * 512], True, True, [b_btok, b_xw], bpst)
                tt(sS[l][:].rearrange("p (h q) -> p h q", h=16), sS[l][:].rearrange("p (h q) -> p h q", h=16),
                   EBL.unsqueeze(2).broadcast_to([128, 16, 64]), ALU.mult, [b_sS[l], b_dec_s], [b_sS[l]])
                tt(sS[l][:], sS[l][:], pst.rearrange("p a b -> p (a b)"), ALU.add, [b_sS[l]] + bpst, [b_sS[l]])
                act(sSb[:], sS[l][:], AF.Copy, [b_sS[l]], [b_sSb])
                yield
                t2, b_t2 = F4[2], b_F4[2]
                tt(t2[:].rearrange("p (h q) -> p h q", h=16), xtok[:].rearrange("p (h q) -> p h q", h=16),
                   bp[:, l, B_SD:B_SD + 16].unsqueeze(2).broadcast_to([128, 16, 64]), ALU.mult, [b_xtok, b_bp], [b_t2])
                tt(t1[:], t1[:], t2[:], ALU.add, [b_t1, b_t2], [b_t1])
                tt(t1[:], t1[:], szs[:, c, :], ALU.mult, [b_t1, b_szs], [b_t1])
                yield
                act(F4[3][:], t1[:], AF.Square, [b_t1], [b_F4[3]])
                rsum(st[:, 0:2], F4[3][:].rearrange("p (h v) -> p h v", h=2), [b_F4[3]], [b_st])
                yield
                r = rstd_from_ss(st[:, 0:2], 2, 512, [])
                yield
                norm_transpose_out(t1[:], [b_t1], r, 2, pp[:, l, P_SNW:P_SNW + 8].unsqueeze(2).broadcast_to([128, 8, 128]),
                                   [b_pp], c, [b_st], A8[2], b_A8[2])
                yield
            tk.dma("sp", sS_d[l], sS[l][:], reads=[b_sS[l]], writes=[b_sSd[l]])

        def outproj(l):
            mb, b_mb = A8[1], b_A8[1]
            act(mb[:], merged[:], AF.Copy, [b_merged], [b_mb])
            for half in range(2):
                sl, bs = slab(wout_d[l][:, half * 512:(half + 1) * 512], 8, 512)
                for c in range(NCH):
                    ps, bps = pbank()
                    for kd in range(8):
                        mm(ps[:, :], mb[:, kd, c * 128:(c + 1) * 128], sl[:, kd, :], kd == 0, kd == 7, [b_mb, bs], bps)
                    tt(hres[:, c, half * 512:(half + 1) * 512], hres[:, c, half * 512:(half + 1) * 512], ps, ALU.add,
                       [b_h] + bps, [b_h])

        def ffn_phase(l):
            rmsnorm_to_uT(l, P_NFFN)
            hid = A8
            j = 0
            while j < 22:
                n = min(4, 22 - j)
                slg, bsg = slab(wup_d[l][:, j * 128:(j + n) * 128], 8, n * 128)
                slv, bsv = slab(wup_d[l][:, DFF + j * 128:DFF + (j + n) * 128], 8, n * 128)
                for jj in range(n):
                    J = j + jj
                    ps, bps = pbank()
                    for kd in range(8):
                        mm(ps[:, 0:TT], slg[:, kd, jj * 128:(jj + 1) * 128], uT[:, kd, :], kd == 0, kd == 7, [bsg, b_uT], bps)
                    Ag, bAg = conv(ps[:, 0:TT], bps, fh[l], b_fh[l], J, 3, P_FCW + J * 3, P_FCB + J, l)
                    act(Ag[:], Ag[:], AF.Silu, [bAg], [bAg])
                    ps2, bps2 = pbank()
                    for kd in range(8):
                        mm(ps2[:, 0:TT], slv[:, kd, jj * 128:(jj + 1) * 128], uT[:, kd, :], kd == 0, kd == 7, [bsv, b_uT], bps2)
                    Av, bAv = conv(ps2[:, 0:TT], bps2, fh[l], b_fh[l], 22 + J, 3, P_FCW + (22 + J) * 3, P_FCB + 22 + J, l)
                    tt(hid[J // 8][:, J % 8, :], Ag[:], Av[:], ALU.mult, [bAg, bAv], [b_A8[J // 8]])
                j += n
            for half in range(2):
                pss = [pbank() for _ in range(NCH)]
                for kg in range(3):
                    nk = 8 if kg < 2 else 6
                    sl, bs = slab(wdn_d[l][kg * 1024:kg * 1024 + nk * 128, half * 512:(half + 1) * 512], nk, 512)
                    for c in range(NCH):
                        ps, bps = pss[c]
                        for kk in range(nk):
                            J = kg * 8 + kk
                            mm(ps[:, :], hid[J // 8][:, J % 8, c * 128:(c + 1) * 128], sl[:, kk, :], J == 0, J == 21,
                               [b_A8[J // 8], bs], bps)
                for c in range(NCH):
                    ps, bps = pss[c]
                    tt(hres[:, c, half * 512:(half + 1) * 512], hres[:, c, half * 512:(half + 1) * 512], ps, ALU.add,
                       [b_h] + bps, [b_h])

        def final_norm_store(t):
            nf, b_nf = F4[3], b_F4[3]
            tk.dma("sp", nf[:], nf_d, writes=[b_nf])
            for c in range(NCH):
                tk.op("dve", lambda e: e.memset(st[:, 0:1], 0.0), writes=[b_st])
                act(F4[0][:], hres[:, c, :], AF.Square, [b_h], [b_F4[0], b_st], accum=st[:, 0:1])
                r = rstd_from_ss(st[:, 0:1], 1, D, [])
                o, b_o = F4[1 + (c % 2)], b_F4[1 + (c % 2)]
                stt(o[:], hres[:, c, :], r, nf[:], ALU.mult, ALU.mult, [b_h, b_st, b_nf], [b_o])
                r0 = t * TT + c * 128
                tk.dma("sp", out_d[r0:r0 + 128, :], o[:], reads=[b_o])

        def emit():
            rr.update({"p1": 0, "a1": 0, "pb": 0, "xs": 0, "acc": 0})
            tk.dma("sp", pp[:], pp_d.rearrange("l p n -> p l n"), writes=[b_pp])
            tk.dma("sp", bp[:], bp_d.rearrange("l p n -> p l n"), writes=[b_bp])
            tk.dma("sp", cst[:], cst_d.rearrange("p (k n) -> p k n", k=NCONST), writes=[b_cst])
            tk.dma("sp", idf[:], idn_d, writes=[b_idf])
            cp(idb[:], idf[:], [b_idf], [b_idb])
            tk.op("dve", lambda e: e.memset(onesb[:], 1.0), writes=[b_onesb])
            tk.op("dve", lambda e: e.memset(gaT[:], 1.0), writes=[b_gaT])
            tk.op("dve", lambda e: e.memset(wab[:], 0.0), writes=[b_wab])
            tk.op("dve", lambda e: e.memset(F4[0][:], 0.0), writes=[b_F4[0]])
            for l in range(NL):
                for t_, b_ in ((mN[l], b_mN[l]), (mh[l], b_mh[l]), (sh[l], b_sh[l]), (fh[l], b_fh[l])):
                    tk.op("dve", lambda e: e.memset(t_[:], 0.0), writes=[b_])
                tk.dma("sp", gS_d[l], F4[0][:], reads=[b_F4[0]], writes=[b_gSd[l]])
                tk.dma("sp", mC_d[l], F4[0][:], reads=[b_F4[0]], writes=[b_mCd[l]])
                tk.dma("sp", sS_d[l], F4[0][:], reads=[b_F4[0]], writes=[b_sSd[l]])
            for t in range(NTILES):
                tk.dma("sp", hres[:], x_d[t * TT:(t + 1) * TT, :].rearrange("(c p) d -> p c d", p=128), writes=[b_h])
                for l in range(NL):
                    rmsnorm_to_uT(l, P_NMIX)
                    gla_proj(l)
                    bg_run(gla_loop(l), lambda: mlstm_proj(l))
                    bg_run(mlstm_loop(l), lambda: (branch(l, 0, A8[2], b_A8[2]), ssd_proj(l)))
                    bg_run(ssd_loop(l), lambda: branch(l, 1, YT2, b_YT2))
                    branch(l, 2, A8[2], b_A8[2])
                    outproj(l)
                    ffn_phase(l)
                final_norm_store(t)
            if not tk.dry:
                for i in range(len(tk.dsem)):
                    if tk.dcnt[i]:
                        tk._wait("sp", (i, tk.dcnt[i]))

        tk.dry = True
        emit()
        tk.dry = False
        emit()
        _bp.last_nins = tk.nins
    return nc


build_program = _bp
build_program.debug = False


def _consts():
    k = np.arange(128)[:, None]
    c = np.arange(128)[None, :]
    tri = (k <= c).astype(np.float32)
    ugt = (k > c).astype(np.float32)
    ones = np.ones((128, 128), np.float32)
    lst = [tri, ugt, tri * (-1.0 / 16.0), ugt * (-1.0 / 16.0), -ugt, ones, -ones, tri * (128.0 ** -0.5)]
    return np.ascontiguousarray(np.concatenate(lst, axis=1).astype(np.float32))


def _fm(v):
    v = np.asarray(v, np.float32)
    return v.reshape(-1, 128).T


def _pack_params(inp, NL):
    pp = np.zeros((NL, 128, NPP), np.float32)
    bp = np.zeros((NL, 128, NBP), np.float32)
    for l in range(NL):
        pp[l, :, P_NMIX:P_NMIX + 8] = _fm(inp["norm_mix"][l])
        pp[l, :, P_NFFN:P_NFFN + 8] = _fm(inp["norm_ffn"][l])
        pp[l, :, P_GNW:P_GNW + 8] = _fm(inp["gla_norm"][l])
        pp[l, :, P_MNW:P_MNW + 8] = _fm(inp["mlstm_norm"][l])
        pp[l, :, P_SNW:P_SNW + 8] = _fm(inp["ssd_norm"][l])
        w = np.asarray(inp["mlstm_conv_w"][l])
        pp[l, :, P_MCW:P_MCW + 32] = np.stack([_fm(w[k]) for k in range(4)], axis=2).reshape(128, 32)
        pp[l, :, P_MCB:P_MCB + 8] = _fm(inp["mlstm_conv_b"][l])
        w = np.asarray(inp["ssd_conv_w"][l])
        pp[l, :, P_SCW:P_SCW + 48] = np.stack([_fm(w[k]) for k in range(4)], axis=2).reshape(128, 48)
        pp[l, :, P_SCB:P_SCB + 12] = _fm(inp["ssd_conv_b"][l])
        w = np.asarray(inp["ffn_conv_w"][l])
        pp[l, :, P_FCW:P_FCW + 132] = np.stack([_fm(w[k]) for k in range(3)], axis=2).reshape(128, 132)
        pp[l, :, P_FCB:P_FCB + 44] = _fm(inp["ffn_conv_b"][l])
        pp[l, :, P_GB:P_GB + 24] = _fm(inp["gate_b"][l])
        row = np.concatenate([inp["mlstm_bi"][l], inp["mlstm_bf"][l], inp["ssd_dt_bias"][l], inp["ssd_a_log"][l],
                              inp["ssd_d"][l]]).astype(np.float32)
        bp[l] = np.broadcast_to(row[None, :], (128, NBP))
    return pp, bp


_CACHE = {}


def run(inputs, NL, B, T, NCH=2, n_cores=None):
    inp = {k: np.asarray(v) for k, v in inputs.items()}
    TT = NCH * 128
    NT = T // TT
    key = (NL, NT, NCH)
    if key not in _CACHE:
        _CACHE[key] = build_program(NL, NT, NCH)
    nc = _CACHE[key]
    pp, bp = _pack_params(inp, NL)
    wab = np.ascontiguousarray(np.concatenate([inp["gla_wa"][:NL], inp["gla_ba"][:NL, None, :]], axis=1).astype(np.float32))
    nf = np.ascontiguousarray(np.broadcast_to(inp["norm_final"].astype(np.float32)[None, :], (128, D)))
    shared = dict(
        w_in=np.ascontiguousarray(inp["w_in"][:NL], np.float32),
        w_branch=np.ascontiguousarray(inp["w_branch"][:NL], np.float32),
        w_out=np.ascontiguousarray(inp["w_out"][:NL], np.float32),
        w_up=np.ascontiguousarray(inp["w_up"][:NL], np.float32),
        w_down=np.ascontiguousarray(inp["w_down"][:NL], np.float32),
        gla_wab=wab, pp=pp, bp=bp, nf=nf, consts=_consts(), ident=np.eye(128, dtype=np.float32),
    )
    in_maps = []
    for b in range(B):
        m = dict(shared)
        m["x"] = np.ascontiguousarray(inp["x"][b, :T], np.float32)
        in_maps.append(m)
    res = run_bass_kernel_spmd(nc, in_maps, core_ids=list(range(B)))
    if _bp.debug:
        run.dbg = np.asarray(res.results[0]["dbg"])
    return np.stack([np.asarray(r["out"]) for r in res.results], axis=0).astype(np.float32)


def kernel(**inputs):
    return run(inputs, NL=4, B=4, T=4096, NCH=2)
```
